# Optimizing a Trainium2 kernel written in Bass

```python
import math
import jax, jax.numpy as jnp
from jax import lax
import numpy as np

D_MODEL = 1024
BATCH = 16
SEQ = 2048
DEPTH = 4

GRID_W = 64
CTX_LEN = 256
HEAD_DIM = 64
A_HEADS = D_MODEL // 128
A_KV_HEADS = 2
A_GROUP = A_HEADS // A_KV_HEADS
CONV_CH = D_MODEL // 4
CONV_WIDTH = 3
C_HEADS = D_MODEL // 256
C_QK_DIM = 32
C_V_DIM = 2 * C_QK_DIM
MIX_A = A_HEADS * HEAD_DIM
MIX_B = CONV_CH
MIX_C = C_HEADS * C_V_DIM
MIX_WIDTH = MIX_A + MIX_B + MIX_C
IN_SPLITS = (MIX_A, A_KV_HEADS * HEAD_DIM, A_KV_HEADS * HEAD_DIM,
             CONV_CH, CONV_CH, CONV_CH,
             C_HEADS * 2 * C_QK_DIM, C_HEADS * 2 * C_QK_DIM, MIX_C)
IN_WIDTH = sum(IN_SPLITS)
IN_OFFSETS = tuple(int(v) for v in np.cumsum(IN_SPLITS)[:-1])
D_FF = ((8 * D_MODEL // 3 + 127) // 128) * 128
N_EXPERTS = 8
TOP_K = 2
N_DENSE = (DEPTH + 1) // 2
N_MOE = DEPTH // 2
Q_BLOCK = 128
ROPE_THETA = 10000.0
EPS = 1e-6

kernel_name = "hybrid_dit_parallel_heads_moe"


def rms_norm(x, g):
    xf = x.astype(jnp.float32)
    y = xf * lax.rsqrt(jnp.mean(xf * xf, axis=-1, keepdims=True) + EPS)
    return (y * g.astype(jnp.float32)).astype(x.dtype)


def rope_1d(x, pos):
    n = x.shape[-1]
    inv = ROPE_THETA ** (-jnp.arange(0, n, 2, dtype=jnp.float32) / n)
    ang = pos.astype(jnp.float32)[:, None] * inv[None, :]
    shape = (1, x.shape[1]) + (1,) * (x.ndim - 3) + (n // 2,)
    cos = jnp.cos(ang).reshape(shape)
    sin = jnp.sin(ang).reshape(shape)
    xf = x.astype(jnp.float32)
    x1, x2 = xf[..., : n // 2], xf[..., n // 2:]
    return jnp.concatenate([x1 * cos - x2 * sin, x2 * cos + x1 * sin], axis=-1).astype(x.dtype)


def axial_rope(x, row, col):
    half = x.shape[-1] // 2
    return jnp.concatenate([rope_1d(x[..., :half], row), rope_1d(x[..., half:], col)], axis=-1)


def gqa_attend(q, k, v):
    s = jnp.einsum('bqkgd,bskd->bkgqs', q, k).astype(jnp.float32) * (q.shape[-1] ** -0.5)
    p = jax.nn.softmax(s, axis=-1).astype(v.dtype)
    return jnp.einsum('bkgqs,bskd->bqkgd', p, v)


def diff_attend(q, k, v, lam):
    s = jnp.einsum('bqhcd,bshcd->bhcqs', q, k).astype(jnp.float32) * (q.shape[-1] ** -0.5)
    p = jax.nn.softmax(s, axis=-1)
    a = p[:, :, 0] - lam * p[:, :, 1]
    return jnp.einsum('bhqs,bshd->bqhd', a.astype(v.dtype), v)


def sweep_query_blocks(fn, q):
    b, n = q.shape[:2]
    qb = jnp.moveaxis(q.reshape((b, n // Q_BLOCK, Q_BLOCK) + q.shape[2:]), 1, 0)
    out = jnp.moveaxis(lax.map(fn, qb), 0, 1)
    return out.reshape((b, n) + out.shape[3:])


def short_conv(z, w):
    pad = (CONV_WIDTH - 1) // 2
    n = z.shape[1]
    zp = jnp.pad(z, ((0, 0), (pad, CONV_WIDTH - 1 - pad), (0, 0)))
    y = w[0] * zp[:, 0:n]
    for j in range(1, CONV_WIDTH):
        y = y + w[j] * zp[:, j:j + n]
    return y


def mixer_inputs(p, a_qg, a_kg, d_qg, d_kg, pos):
    b, n = p.shape[:2]
    aq, ak, av, bu, bb, bc, dq, dk, dv = jnp.split(p, IN_OFFSETS, axis=-1)
    aq = rms_norm(aq.reshape(b, n, A_KV_HEADS, A_GROUP, HEAD_DIM), a_qg)
    ak = rms_norm(ak.reshape(b, n, A_KV_HEADS, HEAD_DIM), a_kg)
    av = av.reshape(b, n, A_KV_HEADS, HEAD_DIM)
    dq = rms_norm(dq.reshape(b, n, C_HEADS, 2, C_QK_DIM), d_qg)
    dk = rms_norm(dk.reshape(b, n, C_HEADS, 2, C_QK_DIM), d_kg)
    dv = dv.reshape(b, n, C_HEADS, C_V_DIM)
    if pos is not None:
        row, col = pos
        aq = axial_rope(aq, row, col)
        ak = axial_rope(ak, row, col)
        dq = axial_rope(dq, row, col)
        dk = axial_rope(dk, row, col)
    return aq, ak, av, bu, bb, bc, dq, dk, dv


def merge_heads(a, bo, d, subln, lam_init, w_o):
    b, n = bo.shape[:2]
    d = rms_norm(d, subln) * (1.0 - lam_init)
    y = jnp.concatenate([a.reshape(b, n, MIX_A), bo, d.reshape(b, n, MIX_C)], axis=-1)
    return y @ w_o


def swiglu(h, wg, wu, wd):
    return (jax.nn.silu(h @ wg) * (h @ wu)) @ wd


def moe_swiglu(h, rw, rb, wg, wu, wd):
    logits = (h @ rw).astype(jnp.float32) + rb.astype(jnp.float32)
    top_v, top_i = lax.top_k(logits, TOP_K)
    gates = jax.nn.softmax(top_v, axis=-1)
    combine = jnp.sum(jax.nn.one_hot(top_i, N_EXPERTS, dtype=jnp.float32) * gates[..., None], axis=-2)
    combine = combine.astype(h.dtype)
    out = jnp.zeros_like(h)
    for e in range(N_EXPERTS):
        out = out + combine[..., e:e + 1] * swiglu(h, wg[e], wu[e], wd[e])
    return out


def channel_mix(h, l, dense_w_gate, dense_w_up, dense_w_down, router_w, router_b,
                moe_w_gate, moe_w_up, moe_w_down):
    i = l // 2
    if l % 2 == 0:
        return swiglu(h, dense_w_gate[i], dense_w_up[i], dense_w_down[i])
    return moe_swiglu(h, router_w[i], router_b[i], moe_w_gate[i], moe_w_up[i], moe_w_down[i])


def setup_inputs(seed: int = 0) -> dict:
    key = jax.random.key(seed)
    ks = jax.random.split(key, 32)
    f32 = jnp.float32
    nrm = lambda k, shape, s: jax.random.normal(k, shape, f32) * s
    D = D_MODEL
    return {
        "x": nrm(ks[0], (BATCH, SEQ, D), 1.0),
        "c": nrm(ks[1], (BATCH, D), 1.0),
        "ctx": nrm(ks[2], (BATCH, CTX_LEN, D), 1.0),
        "c_ctx": nrm(ks[3], (D,), 1.0),
        "w_ada": nrm(ks[4], (DEPTH, D, 6 * D), 0.5 * D ** -0.5),
        "b_ada": nrm(ks[5], (DEPTH, 6 * D), 0.02),
        "norm_mix": 1.0 + nrm(ks[6], (DEPTH, D), 0.02),
        "norm_ffn": 1.0 + nrm(ks[7], (DEPTH, D), 0.02),
        "w_in": nrm(ks[8], (DEPTH, D, IN_WIDTH), D ** -0.5),
        "w_out": nrm(ks[9], (DEPTH, MIX_WIDTH, D), MIX_WIDTH ** -0.5),
        "a_q_gain": 1.0 + nrm(ks[10], (DEPTH, HEAD_DIM), 0.02),
        "a_k_gain": 1.0 + nrm(ks[11], (DEPTH, HEAD_DIM), 0.02),
        "d_q_gain": 1.0 + nrm(ks[12], (DEPTH, C_QK_DIM), 0.02),
        "d_k_gain": 1.0 + nrm(ks[13], (DEPTH, C_QK_DIM), 0.02),
        "conv_w": nrm(ks[14], (DEPTH, CONV_WIDTH, CONV_CH), CONV_WIDTH ** -0.5),
        "diff_lambda": nrm(ks[15], (DEPTH, 4, C_QK_DIM), 0.1),
        "diff_subln": 1.0 + nrm(ks[16], (DEPTH, C_V_DIM), 0.02),
        "dense_w_gate": nrm(ks[17], (N_DENSE, D, D_FF), D ** -0.5),
        "dense_w_up": nrm(ks[18], (N_DENSE, D, D_FF), D ** -0.5),
        "dense_w_down": nrm(ks[19], (N_DENSE, D_FF, D), D_FF ** -0.5),
        "router_w": nrm(ks[20], (N_MOE, D, N_EXPERTS), D ** -0.5),
        "router_b": nrm(ks[21], (N_MOE, N_EXPERTS), 0.01),
        "moe_w_gate": nrm(ks[22], (N_MOE, N_EXPERTS, D, D_FF), D ** -0.5),
        "moe_w_up": nrm(ks[23], (N_MOE, N_EXPERTS, D, D_FF), D ** -0.5),
        "moe_w_down": nrm(ks[24], (N_MOE, N_EXPERTS, D_FF, D), D_FF ** -0.5),
    }


def reference(x, c, ctx, c_ctx, w_ada, b_ada, norm_mix, norm_ffn, w_in, w_out,
              a_q_gain, a_k_gain, d_q_gain, d_k_gain, conv_w, diff_lambda, diff_subln,
              dense_w_gate, dense_w_up, dense_w_down, router_w, router_b,
              moe_w_gate, moe_w_up, moe_w_down):
    n_tok = x.shape[1]
    ROWS = n_tok // GRID_W
    row = jnp.repeat(jnp.arange(ROWS, dtype=jnp.int32), GRID_W)
    col = jnp.tile(jnp.arange(GRID_W, dtype=jnp.int32), ROWS)
    s_lat = jax.nn.silu(c)
    s_ctx = jax.nn.silu(c_ctx)
    xc = ctx
    for l in range(DEPTH):
        last = l == DEPTH - 1
        lam_init = 0.8 - 0.6 * math.exp(-0.3 * l)
        mod = (s_lat @ w_ada[l] + b_ada[l])[:, None, :]
        mod_c = s_ctx @ w_ada[l] + b_ada[l]
        sh1, sc1, g1, sh2, sc2, g2 = jnp.split(mod, 6, axis=-1)
        csh1, csc1, cg1, csh2, csc2, cg2 = jnp.split(mod_c, 6, axis=-1)
        lam_p = diff_lambda[l].astype(jnp.float32)
        lam = (jnp.exp(jnp.sum(lam_p[0] * lam_p[1])) - jnp.exp(jnp.sum(lam_p[2] * lam_p[3]))
               + lam_init)

        h = rms_norm(x, norm_mix[l]) * (1.0 + sc1) + sh1
        hc = rms_norm(xc, norm_mix[l]) * (1.0 + csc1) + csh1
        aq, ak, av, bu, bb, bc, dq, dk, dv = mixer_inputs(
            h @ w_in[l], a_q_gain[l], a_k_gain[l], d_q_gain[l], d_k_gain[l], (row, col))
        caq, cak, cav, cbu, cbb, cbc, cdq, cdk, cdv = mixer_inputs(
            hc @ w_in[l], a_q_gain[l], a_k_gain[l], d_q_gain[l], d_k_gain[l], None)
        ak_all = jnp.concatenate([ak, cak], axis=1)
        av_all = jnp.concatenate([av, cav], axis=1)
        dk_all = jnp.concatenate([dk, cdk], axis=1)
        dv_all = jnp.concatenate([dv, cdv], axis=1)
        a_out = sweep_query_blocks(lambda qb: gqa_attend(qb, ak_all, av_all), aq)
        d_out = sweep_query_blocks(lambda qb: diff_attend(qb, dk_all, dv_all, lam), dq)
        b_out = bb * short_conv(bc * bu, conv_w[l])
        x = x + g1 * merge_heads(a_out, b_out, d_out, diff_subln[l], lam_init, w_out[l])
        if not last:
            ca = gqa_attend(caq, cak, cav)
            cd = diff_attend(cdq, cdk, cdv, lam)
            cb = cbb * short_conv(cbc * cbu, conv_w[l])
            xc = xc + cg1 * merge_heads(ca, cb, cd, diff_subln[l], lam_init, w_out[l])

        h2 = rms_norm(x, norm_ffn[l]) * (1.0 + sc2) + sh2
        x = x + g2 * channel_mix(h2, l, dense_w_gate, dense_w_up, dense_w_down,
                                 router_w, router_b, moe_w_gate, moe_w_up, moe_w_down)
        if not last:
            hc2 = rms_norm(xc, norm_ffn[l]) * (1.0 + csc2) + csh2
            xc = xc + cg2 * channel_mix(hc2, l, dense_w_gate, dense_w_up, dense_w_down,
                                        router_w, router_b, moe_w_gate, moe_w_up, moe_w_down)
    return x
```

```python
import math
from contextlib import ExitStack

import numpy as np
import concourse.bass as bass
import concourse.mybir as mybir
from concourse.bass_utils import run_bass_kernel_spmd

F32 = mybir.dt.float32
BF16 = mybir.dt.bfloat16
ALU = mybir.AluOpType
AF = mybir.ActivationFunctionType
AX = mybir.AxisListType

ENGS = ("pe", "act", "dve", "pool", "sp")
SEM_CAP = 30000
GAP = 4
N_DMA_SEMS = 12


class Res:
    __slots__ = ("w", "r", "name")

    def __init__(self, name=""):
        self.w = {}
        self.r = {}
        self.name = name


class Op:
    __slots__ = ("eng", "fn", "deps", "dma", "sig", "nsig", "slot", "slotval", "key")

    def __init__(self, eng, fn, dma):
        self.eng = eng
        self.fn = fn
        self.dma = dma
        self.deps = []
        self.sig = False
        self.nsig = 0
        self.slot = 0
        self.slotval = 0


class Prog:
    def __init__(self):
        self.ops = {e: [] for e in ENGS}
        self.uid = 0
        self.dmas_since_barrier = []

    def add(self, eng, fn, reads=(), writes=(), dma=False, extra_deps=()):
        op = Op(eng, fn, dma)
        self.uid += 1
        key = ("d", self.uid) if dma else eng
        op.key = key
        deps = {}

        def dep(o, raw):
            if o is op:
                return
            if (not dma) and (not o.dma) and o.eng == eng:
                if eng == "pe":
                    return
            deps[id(o)] = o

        for r in reads:
            for o in r.w.values():
                dep(o, True)
        for w in writes:
            for o in w.r.values():
                dep(o, False)
            for o in w.w.values():
                dep(o, False)
        for o in extra_deps:
            dep(o, True)
        for r in reads:
            r.r[key] = op
        for w in writes:
            if w.r:
                w.w = {key: op}
                w.r = {}
            else:
                w.w[key] = op
        op.deps = list(deps.values())
        for o in op.deps:
            o.sig = True
        self.ops[eng].append(op)
        if dma:
            self.dmas_since_barrier.append(op)
        return op

    def emit(self, nc, final_waits=()):
        nsem = {}
        for e in ENGS:
            c = 0
            for op in self.ops[e]:
                if op.dma:
                    continue
                if op.sig:
                    c += 1
                    op.nsig = c
            nsem[e] = max(1, (c + SEM_CAP - 1) // SEM_CAP)
        for e in ENGS:
            k = 0
            for op in self.ops[e]:
                if op.dma:
                    op.slot = k % N_DMA_SEMS
                    op.slotval = 16 * (k // N_DMA_SEMS + 1)
                    k += 1
        with ExitStack() as st:
            sems = {e: [st.enter_context(nc.semaphore(f"s_{e}_{i}")) for i in range(nsem[e])]
                    for e in ENGS}
            dsems = {e: [st.enter_context(nc.semaphore(f"d_{e}_{i}")) for i in range(N_DMA_SEMS)]
                     for e in ENGS if any(o.dma for o in self.ops[e])}
            block = st.enter_context(nc.Block())

            def target(o):
                if o.dma:
                    return (("d", o.eng, o.slot), dsems[o.eng][o.slot], o.slotval)
                i = (o.nsig - 1) // SEM_CAP
                return (("s", o.eng, i), sems[o.eng][i], o.nsig - i * SEM_CAP)

            def run(e, eng):
                waited = {}
                last_in_slot = {}
                for op in self.ops[e]:
                    tg = [target(o) for o in op.deps]
                    if op.dma and op.slot in last_in_slot:
                        tg.append(target(last_in_slot[op.slot]))
                    best = {}
                    for k, s, v in tg:
                        if waited.get(k, 0) >= v:
                            continue
                        if k not in best or best[k][1] < v:
                            best[k] = (s, v)
                    for k, (s, v) in best.items():
                        eng.wait_ge(s, v)
                        waited[k] = v
                    ins = op.fn(eng)
                    if op.dma:
                        ins.then_inc(dsems[e][op.slot], 16)
                        last_in_slot[op.slot] = op
                    elif op.sig:
                        i = (op.nsig - 1) // SEM_CAP
                        ins.then_inc(sems[e][i], 1)
                for o in final_waits:
                    if o.eng == e:
                        k, s, v = target(o)
                        eng.wait_ge(s, v)

            block.tensor(lambda eng: run("pe", eng))
            block.scalar(lambda eng: run("act", eng))
            block.vector(lambda eng: run("dve", eng))
            block.gpsimd(lambda eng: run("pool", eng))
            block.sync(lambda eng: run("sp", eng))


D = 1024
NLAT = 2048
NCTX = 256
NT = NLAT + NCTX
DEPTH = 4
DFF = 2816
NE = 8
EPS = 1e-6
BLOCKS = [(0, 512), (512, 512), (1024, 512), (1536, 512), (2048, 256)]
NTILE = NT // 128

_off = {}
_c = 0
for _n, _w in (("cT", 24), ("b_ada", 192), ("nmix", 32), ("nffn", 32), ("gqA", 4), ("gkA", 4),
               ("gqD", 4), ("gkD", 4), ("subw", 4), ("conv", 24), ("lam", 512), ("rb", 16),
               ("rw", 128), ("aq_rep", 256), ("ak_rep", 256), ("dq_rep", 128), ("dk_rep", 128)):
    _off[_n] = (_c, _w)
    _c += _w
NS = _c
NCONST = 768


def lam_init_of(l):
    return 0.8 - 0.6 * math.exp(-0.3 * l)


def make_consts():
    c = np.zeros((128, NCONST), np.float32)
    c[:, 0:128] = 1.0
    for p in range(128):
        for m in range(128):
            if p // 64 == m // 64:
                c[p, 128 + m] = 1.0
            if p // 32 == m // 32:
                c[p, 256 + m] = 1.0
    for m in range(128):
        d = m % 32
        if d < 16:
            c[m + 16, 384 + m] = -1.0
        else:
            c[m - 16, 384 + m] = 1.0
        d = m % 16
        if d < 8:
            c[m + 8, 512 + m] = -1.0
        else:
            c[m - 8, 512 + m] = 1.0
    c[:, 640:768] = np.eye(128, dtype=np.float32)
    return c


def make_rope():
    t = np.arange(NLAT)
    row = (t // 64).astype(np.float64)
    col = (t % 64).astype(np.float64)
    out = np.zeros((4, 128, NLAT), np.float64)
    for p in range(128):
        d = p % 64
        pos = row if d < 32 else col
        i = d % 16
        inv = 10000.0 ** (-(2.0 * i) / 32.0)
        out[0, p] = np.cos(pos * inv)
        out[1, p] = np.sin(pos * inv)
        d = p % 32
        pos = row if d < 16 else col
        i = d % 8
        inv = 10000.0 ** (-(2.0 * i) / 16.0)
        out[2, p] = np.cos(pos * inv)
        out[3, p] = np.sin(pos * inv)
    return out.astype(np.float32)


def build_program(n_layers=DEPTH, seqs=(0, 1), debug=False):
    nc = bass.Bass("TRN2", target_bir_lowering=False)

    def din(name, shape):
        return nc.dram_tensor(name, list(shape), F32, kind="ExternalInput").ap()

    x_d = din("x", [2, NLAT, D])
    ctx_d = din("ctx", [2, NCTX, D])
    small_d = din("small", [128, NS])
    const_d = din("consts", [128, NCONST])
    rope_d = din("rope", [4, 128, NLAT])
    w_ada_d = din("w_ada", [DEPTH, D, 6 * D])
    w_in_d = din("w_in", [DEPTH, D, 2304])
    w_out_d = din("w_out", [DEPTH, D, D])
    dwg_d = din("dense_w_gate", [2, D, DFF])
    dwu_d = din("dense_w_up", [2, D, DFF])
    dwd_d = din("dense_w_down", [2, DFF, D])
    mwg_d = din("moe_w_gate", [2, NE, D, DFF])
    mwu_d = din("moe_w_up", [2, NE, D, DFF])
    mwd_d = din("moe_w_down", [2, NE, DFF, D])
    y_d = nc.dram_tensor("y", [2, NLAT, D], F32, kind="ExternalOutput").ap()
    xscr = nc.dram_tensor("xscr", [2, 128, 8, NT], F32, kind=("ExternalOutput" if debug else "Internal")).ap()

    P = Prog()
    R_xscr = [Res("xscr0"), Res("xscr1")]
    out_dmas = []
    tapped = set()

    def tap(name, ap, R, dt=F32):
        if not debug or name in tapped:
            return
        tapped.add(name)
        dd = nc.dram_tensor("dbg_" + name, list(ap.shape), dt, kind="ExternalOutput").ap()
        out_dmas.append(P.add("sp", lambda e: e.dma_start(out=dd, in_=ap), reads=[R], dma=True))

    with ExitStack() as top:
        sbcnt = [0]

        def SB(st, name, shape, dt):
            sbcnt[0] += 1
            return st.enter_context(nc.sbuf_tensor(f"sb{sbcnt[0]}_{name}", list(shape), dt))

        small = SB(top, "small", [128, NS], F32)
        constf = SB(top, "constf", [128, NCONST], F32)
        constb = SB(top, "constb", [128, 640], BF16)
        sT = SB(top, "sT", [128, 8, 3], F32)
        modT = SB(top, "modT", [128, DEPTH, 48, 3], F32)
        AA = SB(top, "AA", [128, DEPTH, 2, 8, 3], F32)
        neglam = SB(top, "neglam", [128, DEPTH], F32)
        nbA = SB(top, "nbA", [128, DEPTH], F32)
        nbD = SB(top, "nbD", [128, DEPTH], F32)
        subws = SB(top, "subws", [128, DEPTH], F32)
        misc = SB(top, "misc", [128, 64], F32)
        bdummy = SB(top, "bdummy", [128, 8], F32)
        banks = [top.enter_context(nc.psum_tensor(f"bank{i}", [128, 512], F32)) for i in range(8)]
        R_bank = [Res(f"bank{i}") for i in range(8)]
        R_small, R_constf, R_constb, R_sT, R_modT, R_par = (Res() for _ in range(6))
        R_bar = {e: Res("bar_" + e) for e in ENGS}

        def sm(name, lo=0, hi=None):
            o, w = _off[name]
            hi = w if hi is None else hi
            return small[:, o + lo:o + hi]

        ones_b = constb[:, 0:128]
        bd64_b = constb[:, 128:256]
        bd32_b = constb[:, 256:384]
        rotA_b = constb[:, 384:512]
        rotD_b = constb[:, 512:640]
        identf = constf[:, 640:768]

        def barrier():
            dl = list(P.dmas_since_barrier)
            P.dmas_since_barrier = []
            P.add("pe", lambda e: e.matmul(banks[7][0:1, 0:1], lhsT=constb[0:1, 0:1], rhs=constb[0:1, 0:1],
                                           start=True, stop=True), reads=[R_constb], writes=[R_bar["pe"], R_bank[7]])
            P.add("act", lambda e: e.activation(out=bdummy[0:1, 0:1], in_=bdummy[0:1, 0:1], func=AF.Copy),
                  writes=[R_bar["act"]])
            P.add("dve", lambda e: e.tensor_copy(out=bdummy[0:1, 1:2], in_=bdummy[0:1, 1:2]), writes=[R_bar["dve"]])
            P.add("pool", lambda e: e.memset(bdummy[0:1, 2:3], 0.0), writes=[R_bar["pool"]])
            P.add("sp", lambda e: e.dma_start(out=bdummy[0:1, 4:8], in_=const_d[0:1, 0:4]),
                  writes=[R_bar["sp"]], dma=True, extra_deps=dl)
            allb = list(R_bar.values())
            P.add("pe", lambda e: e.matmul(banks[7][0:1, 0:1], lhsT=constb[0:1, 0:1], rhs=constb[0:1, 0:1],
                                           start=True, stop=True), reads=allb + [R_constb], writes=[R_bank[7]])
            P.add("act", lambda e: e.activation(out=bdummy[0:1, 0:1], in_=bdummy[0:1, 0:1], func=AF.Copy), reads=allb)
            P.add("dve", lambda e: e.tensor_copy(out=bdummy[0:1, 1:2], in_=bdummy[0:1, 1:2]), reads=allb)
            P.add("pool", lambda e: e.memset(bdummy[0:1, 2:3], 0.0), reads=allb)
            P.add("sp", lambda e: e.dma_start(out=bdummy[0:1, 4:8], in_=const_d[0:1, 0:4]), reads=allb, dma=True)
            P.dmas_since_barrier = []

        P.add("sp", lambda e: e.dma_start(out=small[:], in_=small_d), writes=[R_small], dma=True)
        P.add("sp", lambda e: e.dma_start(out=constf[:], in_=const_d), writes=[R_constf], dma=True)
        P.add("dve", lambda e: e.tensor_copy(out=constb[:], in_=constf[:, 0:640]), reads=[R_constf], writes=[R_constb])
        P.add("pool", lambda e: e.memset(bdummy[:], 0.0), writes=list(R_bar.values()))
        P.add("act", lambda e: e.activation(out=sT[:].rearrange("p k j -> p (k j)"), in_=sm("cT"), func=AF.Silu),
              reads=[R_small], writes=[R_sT])

        def pre_params():
            lam4 = sm("lam").rearrange("p (l f d) -> p l f d", l=4, f=4)
            prod = misc[:, 0:16]
            tmp = SB(top, "lamtmp", [128, 4, 2, 32], F32)
            P.add("dve", lambda e: e.tensor_tensor(out=tmp[:], in0=lam4[:, :, 0:4:2, :], in1=lam4[:, :, 1:4:2, :],
                                                   op=ALU.mult), reads=[R_small], writes=[R_par])
            P.add("dve", lambda e: e.tensor_reduce(out=misc[:, 0:8], in_=tmp[:].rearrange("p l f d -> p (l f) d"),
                                                   axis=AX.X, op=ALU.add), reads=[R_par], writes=[R_par])
            P.add("act", lambda e: e.activation(out=misc[:, 8:16], in_=misc[:, 0:8], func=AF.Exp),
                  reads=[R_par], writes=[R_par])
            ev = misc[:, 8:16].rearrange("p (l f) -> p l f", f=2)
            P.add("dve", lambda e: e.tensor_tensor(out=neglam[:], in0=ev[:, :, 1], in1=ev[:, :, 0], op=ALU.subtract),
                  reads=[R_par], writes=[R_par])
            for l in range(DEPTH):
                P.add("dve", lambda e, l=l: e.tensor_scalar_add(out=neglam[:, l:l + 1], in0=neglam[:, l:l + 1],
                                                                scalar1=-lam_init_of(l)), reads=[R_par], writes=[R_par])
                P.add("dve", lambda e, l=l: e.tensor_scalar_mul(out=subws[:, l:l + 1], in0=sm("subw", l, l + 1),
                                                                scalar1=1.0 - lam_init_of(l)),
                      reads=[R_small], writes=[R_par])
            for j, (nm, w) in enumerate((("aq_rep", 64), ("ak_rep", 64), ("dq_rep", 32), ("dk_rep", 32))):
                P.add("dve", lambda e, nm=nm, w=w, j=j: e.reduce_max(
                    out=misc[:, 16 + 4 * j:20 + 4 * j], in_=sm(nm).rearrange("p (l d) -> p l d", d=w),
                    axis=AX.X, apply_absolute_value=True), reads=[R_small], writes=[R_par])
            P.add("dve", lambda e: e.scalar_tensor_tensor(out=nbA[:], in0=misc[:, 16:20], scalar=-8.0, in1=misc[:, 20:24],
                                                          op0=ALU.mult, op1=ALU.mult), reads=[R_par], writes=[R_par])
            P.add("dve", lambda e: e.scalar_tensor_tensor(out=nbD[:], in0=misc[:, 24:28], scalar=-math.sqrt(32.0),
                                                          in1=misc[:, 28:32], op0=ALU.mult, op1=ALU.mult),
                  reads=[R_par], writes=[R_par])

        pre_params()

        with ExitStack() as st:
            wada = [SB(st, f"wada{i}", [128, 8, 768], BF16) for i in range(2)]
            sTb = SB(st, "sTb", [128, 8, 3], BF16)
            P.add("dve", lambda e: e.tensor_copy(out=sTb[:], in_=sT[:]), reads=[R_sT], writes=[R_sT])
            R_wada = [Res(), Res()]
            cnt = 0
            for l in range(n_layers):
                wv = w_ada_d[l].rearrange("(k p) n -> p k n", p=128)
                for g in range(8):
                    a = cnt % 2
                    cnt += 1
                    P.add("pool", lambda e, a=a, wv=wv, g=g: e.dma_start(out=wada[a][:], in_=wv[:, :, g * 768:(g + 1) * 768]),
                          writes=[R_wada[a]], dma=True)
                    bk = banks[g % 2]
                    for jj in range(6):
                        for k in range(8):
                            P.add("pe", lambda e, a=a, jj=jj, k=k, bk=bk: e.matmul(
                                bk[:, jj * 4:jj * 4 + 3], lhsT=wada[a][:, k, jj * 128:(jj + 1) * 128], rhs=sTb[:, k, :],
                                start=(k == 0), stop=(k == 7)), reads=[R_wada[a], R_sT], writes=[R_bank[g % 2]])
                    bo, _ = _off["b_ada"]
                    P.add("dve", lambda e, l=l, g=g, bk=bk, bo=bo: e.tensor_tensor(
                        out=modT[:, l, g * 6:(g + 1) * 6, :],
                        in0=bk[:, 0:24].rearrange("p (j c) -> p j c", c=4)[:, :, 0:3],
                        in1=small[:, bo + l * 48 + g * 6:bo + l * 48 + g * 6 + 6].unsqueeze(2).to_broadcast([128, 6, 3]),
                        op=ALU.add), reads=[R_bank[g % 2], R_small], writes=[R_modT])
                for which, (nm, base) in enumerate((("nmix", 8), ("nffn", 32))):
                    P.add("dve", lambda e, l=l, which=which, nm=nm, base=base: e.scalar_tensor_tensor(
                        out=AA[:, l, which, :, :], in0=modT[:, l, base:base + 8, :], scalar=1.0,
                        in1=sm(nm, l * 8, l * 8 + 8).unsqueeze(2).to_broadcast([128, 8, 3]),
                        op0=ALU.add, op1=ALU.mult), reads=[R_modT, R_small], writes=[R_modT])
            tap("modT", modT[:].rearrange("p l j c -> p (l j c)"), R_modT)
            tap("AA", AA[:].rearrange("p l w k c -> p (l w k c)"), R_modT)
            tap("neglam", neglam[:], R_par)
            tap("nbA", nbA[:], R_par)
            tap("subws", subws[:], R_par)
            barrier()

        def run(gen):
            for _ in gen:
                pass

        def rr(gens):
            gens = list(gens)
            while gens:
                for g in list(gens):
                    try:
                        next(g)
                    except StopIteration:
                        gens.remove(g)

        def norm_block(st_bufs, xget, R_x, nb, l, which, bsel, hout, R_h, h32=None):
            sqb, R_sq, rs, R_rs, tn, R_tn, bank_i = st_bufs
            bk = banks[bank_i]
            def _sq(k):
                a = k % 2
                P.add("act", lambda e: e.activation(out=sqb[a][:, :nb], in_=xget(k), func=AF.Square),
                      reads=[R_x], writes=[R_sq[a]])

            def _mm(k):
                a = k % 2
                P.add("pe", lambda e: e.matmul(bk[:, :nb], lhsT=ones_b, rhs=sqb[a][:, :nb],
                                               start=(k == 0), stop=(k == 7)),
                      reads=[R_sq[a], R_constb], writes=[R_bank[bank_i]])
            _sq(0)
            yield
            _sq(1)
            for _ in range(GAP):
                yield
            for k in range(8):
                _mm(k)
                yield
                if k + 2 < 8:
                    _sq(k + 2)
                    yield
                    yield
            P.add("act", lambda e: e.activation(out=rs[:, :nb], in_=bk[:, :nb], func=AF.Ln, scale=1.0 / D, bias=EPS),
                  reads=[R_bank[bank_i]], writes=[R_rs])
            P.add("act", lambda e: e.activation(out=rs[:, :nb], in_=rs[:, :nb], func=AF.Exp, scale=-0.5),
                  reads=[R_rs], writes=[R_rs])
            shb = 0 if which == 0 else 24
            yield
            for k in range(8):
                a = k % 2
                P.add("dve", lambda e, k=k, a=a: e.tensor_tensor(out=tn[a][:, :nb], in0=xget(k), in1=rs[:, :nb], op=ALU.mult),
                      reads=[R_x, R_rs], writes=[R_tn[a]])
                if h32 is None:
                    P.add("dve", lambda e, k=k, a=a: e.tensor_scalar(
                        out=hout(k), in0=tn[a][:, :nb], scalar1=AA[:, l, which, k, bsel:bsel + 1],
                        scalar2=modT[:, l, shb + k, bsel:bsel + 1], op0=ALU.mult, op1=ALU.add),
                        reads=[R_tn[a], R_modT], writes=[R_h])
                else:
                    P.add("dve", lambda e, k=k, a=a: e.tensor_scalar(
                        out=tn[a][:, :nb], in0=tn[a][:, :nb], scalar1=AA[:, l, which, k, bsel:bsel + 1],
                        scalar2=modT[:, l, shb + k, bsel:bsel + 1], op0=ALU.mult, op1=ALU.add),
                        reads=[R_tn[a], R_modT], writes=[R_tn[a]])
                    P.add("act", lambda e, k=k, a=a: e.activation(out=hout(k), in_=tn[a][:, :nb], func=AF.Copy),
                          reads=[R_tn[a]], writes=[R_h])
                    h32(k, tn[a], R_tn[a])
                yield

        def ingest(s):
            with ExitStack() as st:
                xt = [SB(st, f"ixt{i}", [128, D], F32) for i in range(2)]
                xT = [SB(st, f"ixT{i}", [128, 8, 128], F32) for i in range(2)]
                R_xt = [Res(), Res()]
                R_xT = [Res(), Res()]
                for i in range(NTILE):
                    a = i % 2
                    src = x_d[s, i * 128:(i + 1) * 128, :] if i < 16 else ctx_d[s, (i - 16) * 128:(i - 15) * 128, :]
                    P.add("sp", lambda e, a=a, src=src: e.dma_start(out=xt[a][:], in_=src), writes=[R_xt[a]], dma=True)
                    for k in range(8):
                        bi = k // 4
                        P.add("pe", lambda e, a=a, k=k, bi=bi: e.transpose(
                            banks[bi][:, (k % 4) * 128:(k % 4 + 1) * 128], xt[a][:, k * 128:(k + 1) * 128], identf),
                            reads=[R_xt[a], R_constf], writes=[R_bank[bi]])
                    P.add("act", lambda e, a=a: e.activation(out=xT[a][:, 0:4, :].rearrange("p k t -> p (k t)"),
                                                             in_=banks[0][:], func=AF.Copy),
                          reads=[R_bank[0]], writes=[R_xT[a]])
                    P.add("dve", lambda e, a=a: e.tensor_copy(out=xT[a][:, 4:8, :].rearrange("p k t -> p (k t)"),
                                                              in_=banks[1][:]),
                          reads=[R_bank[1]], writes=[R_xT[a]])
                    P.add("sp", lambda e, a=a, i=i: e.dma_start(out=xscr[s][:, :, i * 128:(i + 1) * 128], in_=xT[a][:]),
                          reads=[R_xT[a]], writes=[R_xscr[s]], dma=True)
                barrier()

        def mixer(s, l, last):
            with ExitStack() as st:
                wA = SB(st, "wA", [128, 8, 2048], BF16)
                w1 = wA
                w2 = wA[:, :, 0:1024]
                wo = wA[:, :, 1024:2048]
                kA = SB(st, "kA", [128, 2, NT], BF16)
                kD = SB(st, "kD", [128, 2, NT], BF16)
                zb = SB(st, "zb", [128, 2, NT + 4], BF16)
                Vb = SB(st, "Vb", [128, NTILE, 11, 64], BF16)
                xb = [SB(st, f"xb{i}", [128, 8, 512], F32) for i in range(1)]
                hb = [SB(st, f"hb{i}", [128, 8, 512], BF16) for i in range(1)]
                tabs = [SB(st, f"tabs{i}", [128, 4, 512], F32) for i in range(1)]
                q6 = [SB(st, f"q6{i}", [128, 6, 512], BF16) for i in range(2)]
                yb = [SB(st, f"yb{i}", [128, 8, 512], BF16) for i in range(2)]
                PT = [SB(st, f"PT{i}", [128, 512], BF16) for i in range(4)]
                sqb = [SB(st, f"sqb{i}", [128, 512], BF16) for i in range(2)]
                rs = SB(st, "rs", [128, 512], F32)
                tn = [SB(st, f"tn{i}", [128, 512], F32) for i in range(2)]
                t0 = [SB(st, f"t0{i}", [128, 512], F32) for i in range(2)]
                knb = [SB(st, f"knb{i}", [128, 512], BF16) for i in range(2)]
                rs2 = [SB(st, f"rs2{i}", [128, 512], F32) for i in range(2)]
                c1 = [SB(st, f"c1{i}", [128, 512], F32) for i in range(2)]
                c2 = [SB(st, f"c2{i}", [128, 512], F32) for i in range(2)]
                rl = [SB(st, f"rl{i}", [128, 512], F32) for i in range(3)]
                dt_ = [SB(st, f"dt{i}", [128, 512], F32) for i in range(2)]
                dbuf = SB(st, "dbuf", [128, 2, 512], F32)
                R_w1, R_kv, R_rs, R_dbuf = (Res() for _ in range(4))
                R_q6 = [Res(), Res()]
                R_yb = [Res(), Res()]
                R_w2 = R_w1
                R_wo = R_w1
                R_xb = [Res()]
                R_hb = [Res()]
                R_tabs = [Res()]
                R_PT = [Res() for _ in range(4)]
                R_sq = [Res(), Res()]
                R_tn = [Res(), Res()]
                R_t0 = [Res(), Res()]
                R_knb = [Res(), Res()]
                R_rs2 = [Res(), Res()]
                R_c1 = [Res(), Res()]
                R_c2 = [Res(), Res()]
                R_rl = [Res() for _ in range(3)]
                R_dt = [Res(), Res()]
                nbufs = (sqb, R_sq, rs, R_rs, tn, R_tn, 0)

                wv = w_in_d[l].rearrange("(k p) n -> p k n", p=128)

                def wload(dst, R, off, lo, hi):
                    P.add("pool", lambda e: e.dma_start(out=dst[:, :, off:off + (hi - lo)], in_=wv[:, :, lo:hi]),
                          writes=[R], dma=True)
                wload(w1, R_w1, 0, 512, 576)
                wload(w1, R_w1, 64, 512, 576)
                wload(w1, R_w1, 128, 576, 640)
                wload(w1, R_w1, 192, 576, 640)
                wload(w1, R_w1, 256, 1792, 2048)
                wload(w1, R_w1, 512, 768, 1024)
                wload(w1, R_w1, 768, 1280, 1536)
                wload(w1, R_w1, 1024, 640, 768)
                wload(w1, R_w1, 1152, 2048, 2304)

                for blk in (0, 2, 4, 6, 9):
                    P.add("pool", lambda e, blk=blk: e.memset(Vb[:, :, blk, :], 1.0), writes=[R_kv])
                for (a_, b_) in ((0, 1), (2049, 2051), (2307, 2308)):
                    P.add("pool", lambda e, a_=a_, b_=b_: e.memset(zb[:, :, a_:b_], 0.0), writes=[R_kv])

                def zcol(t):
                    return t + 1 if t < NLAT else t + 3

                bsel_of = lambda b: (s if b < 4 else 2)
                xcnt = [0]

                def load_x(b):
                    t_0, nb = BLOCKS[b]
                    a = 0
                    P.add("sp", lambda e: e.dma_start(out=xb[a][:, :, :nb], in_=xscr[s][:, :, t_0:t_0 + nb]),
                          reads=[R_xscr[s]], writes=[R_xb[a]], dma=True)
                    return a

                tcnt = [0]

                def load_tabs(b):
                    t_0, nb = BLOCKS[b]
                    a = 0
                    P.add("sp", lambda e: e.dma_start(out=tabs[a][:, :, :nb],
                                                      in_=rope_d[:, :, t_0:t_0 + nb].rearrange("f p t -> p f t")),
                          writes=[R_tabs[a]], dma=True)
                    return a

                pcnt = [0]

                def qk_post(bi, nb, kind, gain_ap, rope, ta, out_ap, R_out):
                    i = pcnt[0] % 2
                    pcnt[0] += 1
                    bk = banks[bi]
                    hd, bdm, rotm, tb = (64, bd64_b, rotA_b, 0) if kind == "A" else (32, bd32_b, rotD_b, 2)
                    P.add("act", lambda e: e.mul(out=t0[i][:, :nb], in_=bk[:, :nb], mul=gain_ap),
                          reads=[R_bank[bi], R_small], writes=[R_t0[i]])
                    yield
                    P.add("act", lambda e: e.activation(out=sqb[i][:, :nb], in_=bk[:, :nb], func=AF.Square),
                          reads=[R_bank[bi]], writes=[R_sq[i]])
                    for _ in range(GAP):
                        yield
                    P.add("pe", lambda e: e.matmul(bk[:, :nb], lhsT=bdm, rhs=sqb[i][:, :nb], start=True, stop=True),
                          reads=[R_sq[i], R_constb, R_t0[i]], writes=[R_bank[bi]])
                    yield
                    P.add("act", lambda e: e.activation(out=rs2[i][:, :nb], in_=bk[:, :nb], func=AF.Ln, scale=1.0 / hd, bias=EPS),
                          reads=[R_bank[bi]], writes=[R_rs2[i]])
                    yield
                    P.add("act", lambda e: e.activation(out=rs2[i][:, :nb], in_=rs2[i][:, :nb], func=AF.Exp, scale=-0.5),
                          reads=[R_rs2[i]], writes=[R_rs2[i]])
                    yield
                    if not rope:
                        P.add("dve", lambda e: e.tensor_tensor(out=out_ap, in0=t0[i][:, :nb], in1=rs2[i][:, :nb], op=ALU.mult),
                              reads=[R_t0[i], R_rs2[i]], writes=[R_out])
                        yield
                        return
                    P.add("dve", lambda e: e.tensor_tensor(out=knb[i][:, :nb], in0=t0[i][:, :nb], in1=rs2[i][:, :nb], op=ALU.mult),
                          reads=[R_t0[i], R_rs2[i]], writes=[R_knb[i]])
                    for _ in range(GAP):
                        yield
                    P.add("pe", lambda e: e.matmul(bk[:, :nb], lhsT=rotm, rhs=knb[i][:, :nb], start=True, stop=True),
                          reads=[R_knb[i], R_constb, R_rs2[i]], writes=[R_bank[bi]])
                    yield
                    P.add("dve", lambda e: e.tensor_tensor(out=c1[i][:, :nb], in0=knb[i][:, :nb], in1=tabs[ta][:, tb, :nb], op=ALU.mult),
                          reads=[R_knb[i], R_tabs[ta]], writes=[R_c1[i]])
                    yield
                    P.add("dve", lambda e: e.tensor_tensor(out=c2[i][:, :nb], in0=bk[:, :nb], in1=tabs[ta][:, tb + 1, :nb], op=ALU.mult),
                          reads=[R_bank[bi], R_tabs[ta]], writes=[R_c2[i]])
                    yield
                    P.add("dve", lambda e: e.tensor_tensor(out=out_ap, in0=c1[i][:, :nb], in1=c2[i][:, :nb], op=ALU.add),
                          reads=[R_c1[i], R_c2[i]], writes=[R_out])
                    yield

                def proj(wt, R_w, col, ha, nb, bi):
                    for k in range(8):
                        P.add("pe", lambda e, k=k: e.matmul(banks[bi][:, :nb], lhsT=wt[:, k, col:col + 128], rhs=hb[ha][:, k, :nb],
                                                            start=(k == 0), stop=(k == 7)),
                              reads=[R_w, R_hb[ha]], writes=[R_bank[bi]])
                        if k == 3:
                            yield
                    yield

                pj = [1, 2, 3, 4, 5, 6, 7]
                pjc = [0]

                def nextbank():
                    b_ = pj[pjc[0] % len(pj)]
                    pjc[0] += 1
                    return b_

                def pass1_block(b):
                    t_0, nb = BLOCKS[b]
                    xa = load_x(b)
                    rope = b < 4
                    ta = load_tabs(b) if rope else 0
                    ha = 0
                    bsel = bsel_of(b)
                    run(norm_block(nbufs, lambda k, xa=xa, nb=nb: xb[xa][:, k, :nb], R_xb[xa], nb, l, 0, bsel,
                                   lambda k, ha=ha, nb=nb: hb[ha][:, k, :nb], R_hb[ha]))
                    for c0 in (0, 2):
                        gens = []
                        for c in (c0, c0 + 1):
                            bi = nextbank()
                            run(proj(w1, R_w1, c * 128, ha, nb, bi))
                            if c < 2:
                                gens.append(qk_post(bi, nb, "A", sm("gkA", l, l + 1), rope, ta, kA[:, c, t_0:t_0 + nb], R_kv))
                            else:
                                gens.append(qk_post(bi, nb, "D", sm("gkD", l, l + 1), rope, ta, kD[:, c - 2, t_0:t_0 + nb], R_kv))
                        rr(gens)
                    for c in range(2):
                        bu_i = nextbank()
                        run(proj(w1, R_w1, 512 + c * 128, ha, nb, bu_i))
                        i = pcnt[0] % 2
                        pcnt[0] += 1
                        P.add("act", lambda e, i=i, bu_i=bu_i, nb=nb: e.activation(out=t0[i][:, :nb], in_=banks[bu_i][:, :nb], func=AF.Copy),
                              reads=[R_bank[bu_i]], writes=[R_t0[i]])
                        bc_i = nextbank()
                        run(proj(w1, R_w1, 768 + c * 128, ha, nb, bc_i))
                        zc = zcol(t_0)
                        P.add("dve", lambda e, i=i, bc_i=bc_i, nb=nb, c=c, zc=zc: e.tensor_tensor(
                            out=zb[:, c, zc:zc + nb], in0=banks[bc_i][:, :nb], in1=t0[i][:, :nb], op=ALU.mult),
                            reads=[R_bank[bc_i], R_t0[i]], writes=[R_kv])
                    for j in range(nb // 128):
                        ti = t_0 // 128 + j
                        bi = nextbank()
                        for k in range(8):
                            P.add("pe", lambda e, k=k, j=j, bi=bi, ha=ha: e.matmul(
                                banks[bi][:, 0:384], lhsT=hb[ha][:, k, j * 128:(j + 1) * 128], rhs=w1[:, k, 1024:1408],
                                start=(k == 0), stop=(k == 7)), reads=[R_w1, R_hb[ha]], writes=[R_bank[bi]])
                        P.add("act", lambda e, ti=ti, bi=bi: e.activation(
                            out=Vb[:, ti, 1:5:2, :], in_=banks[bi][:, 0:128].rearrange("p (h d) -> p h d", d=64), func=AF.Copy),
                            reads=[R_bank[bi]], writes=[R_kv])
                        P.add("act", lambda e, ti=ti, bi=bi: e.activation(
                            out=Vb[:, ti, 5:9:2, :], in_=banks[bi][:, 128:256].rearrange("p (h d) -> p h d", d=64), func=AF.Copy),
                            reads=[R_bank[bi]], writes=[R_kv])
                        P.add("act", lambda e, ti=ti, bi=bi: e.activation(
                            out=Vb[:, ti, 8:11:2, :], in_=banks[bi][:, 256:384].rearrange("p (h d) -> p h d", d=64), func=AF.Copy),
                            reads=[R_bank[bi]], writes=[R_kv])

                for b in range(5):
                    pass1_block(b)
                tap("kA", kA[:].rearrange("p c t -> p (c t)"), R_kv, BF16)
                tap("kD", kD[:].rearrange("p c t -> p (c t)"), R_kv, BF16)
                tap("zb", zb[:].rearrange("p c t -> p (c t)"), R_kv, BF16)
                tap("Vb", Vb[:].rearrange("p t b d -> p (t b d)"), R_kv, BF16)
                wload(wA, R_w1, 0, 0, 512)
                wload(wA, R_w1, 512, 1536, 1792)
                wload(wA, R_w1, 768, 1024, 1280)
                P.add("pool", lambda e: e.dma_start(out=wA[:, :, 1024:2048], in_=w_out_d[l].rearrange("(k p) n -> p k n", p=128)),
                      writes=[R_w1], dma=True)
                sbanks = [2, 3, 4]
                obanks = [5, 6, 7]
                pj2 = [0, 1]
                nblocks = 4 if last else 5
                def prologue(b):
                    t_0, nb = BLOCKS[b]
                    qa = b % 2
                    xa = load_x(b)
                    rope = b < 4
                    ta = load_tabs(b) if rope else 0
                    ha = 0
                    bsel = bsel_of(b)
                    yield
                    yield from norm_block(nbufs, lambda k: xb[xa][:, k, :nb], R_xb[xa], nb, l, 0, bsel,
                                          lambda k: hb[ha][:, k, :nb], R_hb[ha])
                    for _ in range(GAP):
                        yield
                    for c in range(6):
                        bi = pj2[c % 2]
                        yield from proj(w2, R_w2, c * 128, ha, nb, bi)
                        yield
                        if c < 4:
                            yield from qk_post(bi, nb, "A", sm("gqA", l, l + 1), rope, ta, q6[qa][:, c, :nb], R_q6[qa])
                        else:
                            yield from qk_post(bi, nb, "D", sm("gqD", l, l + 1), rope, ta, q6[qa][:, c, :nb], R_q6[qa])
                    tap("hb", hb[ha][:].rearrange("p k t -> p (k t)"), R_hb[ha], BF16)
                    tap("q6", q6[qa][:].rearrange("p k t -> p (k t)"), R_q6[qa], BF16)
                    co, _ = _off["conv"]
                    zc = zcol(t_0)
                    for c in range(2):
                        bi = pj2[c % 2]
                        yield from proj(w2, R_w2, 768 + c * 128, ha, nb, bi)
                        i = pcnt[0] % 2
                        pcnt[0] += 1
                        wcol = lambda j, c=c: small[:, co + l * 6 + j * 2 + c:co + l * 6 + j * 2 + c + 1]
                        P.add("dve", lambda e, i=i, c=c, wcol=wcol: e.tensor_scalar(
                            out=c1[i][:, :nb], in0=zb[:, c, zc:zc + nb], scalar1=wcol(1), scalar2=None, op0=ALU.mult),
                            reads=[R_kv, R_small], writes=[R_c1[i]])
                        yield
                        P.add("dve", lambda e, i=i, c=c, wcol=wcol: e.scalar_tensor_tensor(
                            out=c2[i][:, :nb], in0=zb[:, c, zc - 1:zc - 1 + nb], scalar=wcol(0), in1=c1[i][:, :nb],
                            op0=ALU.mult, op1=ALU.add), reads=[R_kv, R_small, R_c1[i]], writes=[R_c2[i]])
                        yield
                        P.add("dve", lambda e, i=i, c=c, wcol=wcol: e.scalar_tensor_tensor(
                            out=c1[i][:, :nb], in0=zb[:, c, zc + 1:zc + 1 + nb], scalar=wcol(2), in1=c2[i][:, :nb],
                            op0=ALU.mult, op1=ALU.add), reads=[R_kv, R_small, R_c2[i]], writes=[R_c1[i]])
                        yield
                        P.add("dve", lambda e, i=i, c=c, bi=bi: e.tensor_tensor(
                            out=yb[qa][:, 4 + c, :nb], in0=banks[bi][:, :nb], in1=c1[i][:, :nb], op=ALU.mult),
                            reads=[R_bank[bi], R_c1[i]], writes=[R_yb[qa]])
                        yield

                def epilogue(b):
                    t_0, nb = BLOCKS[b]
                    qa = b % 2
                    bsel = bsel_of(b)
                    xa = load_x(b)
                    yield
                    for oc in range(8):
                        bi = pj2[oc % 2]
                        for c in range(8):
                            P.add("pe", lambda e, c=c, oc=oc, bi=bi: e.matmul(banks[bi][:, :nb], lhsT=wo[:, c, oc * 128:(oc + 1) * 128],
                                                                              rhs=yb[qa][:, c, :nb], start=(c == 0), stop=(c == 7)),
                                  reads=[R_wo, R_yb[qa]], writes=[R_bank[bi]])
                            if c == 3:
                                yield
                        yield
                        yield
                        P.add("dve", lambda e, oc=oc, bi=bi: e.scalar_tensor_tensor(
                            out=xb[xa][:, oc, :nb], in0=banks[bi][:, :nb], scalar=modT[:, l, 16 + oc, bsel:bsel + 1],
                            in1=xb[xa][:, oc, :nb], op0=ALU.mult, op1=ALU.add),
                            reads=[R_bank[bi], R_xb[xa], R_modT], writes=[R_xb[xa]])
                        yield
                    P.add("sp", lambda e: e.dma_start(out=xscr[s][:, :, t_0:t_0 + nb], in_=xb[xa][:, :, :nb]),
                          reads=[R_xb[xa]], writes=[R_xscr[s]], dma=True)
                    tap("x1", xb[xa][:].rearrange("p k t -> p (k t)"), R_xb[xa])
                    yield

                def attention(b, pending):
                    t_0, nb = BLOCKS[b]
                    qa = b % 2
                    q6c, ybc, R_q6c, R_ybc = q6[qa], yb[qa], R_q6[qa], R_yb[qa]

                    def pump():
                        while pending:
                            try:
                                next(pending[0])
                                return
                            except StopIteration:
                                pending.pop(0)

                    ktiles = list(range(NTILE)) if b < 4 else [16, 17]
                    groups = []
                    for h in range(8):
                        groups.append(("A", h, 0))
                    for h in range(4):
                        groups.append(("D", h, 0))
                        groups.append(("D", h, 1))
                    steps = [(gi, kt) for gi in range(len(groups)) for kt in ktiles]
                    obank_of = {}
                    rl_cnt = [0]

                    def emit_S(idx):
                        gi, kt = steps[idx]
                        kind, h, cc = groups[gi]
                        sb_i = sbanks[idx % 3]
                        if kind == "A":
                            par = h % 2
                            p0, pn = par * 64, 64
                            kv = h // 4
                            lhs = kA[p0:p0 + pn, kv, kt * 128:(kt + 1) * 128]
                            rhs = q6c[p0:p0 + pn, h // 2, :nb]
                        else:
                            p0, pn = (h % 2) * 64 + cc * 32, 32
                            lhs = kD[p0:p0 + pn, h // 2, kt * 128:(kt + 1) * 128]
                            rhs = q6c[p0:p0 + pn, 4 + h // 2, :nb]
                        kw = {}
                        if p0 == 96:
                            kw["tile_position"] = (96, 0)
                        P.add("pe", lambda e: e.matmul(banks[sb_i][:, :nb], lhsT=lhs, rhs=rhs, start=True, stop=True, **kw),
                              reads=[R_kv, R_q6c], writes=[R_bank[sb_i]])
                        pt_i = idx % 4
                        if kind == "A":
                            sc, bias = 0.125, nbA[:, l:l + 1]
                        else:
                            sc, bias = 32.0 ** -0.5, nbD[:, l:l + 1]
                        P.add("act", lambda e: e.activation(out=PT[pt_i][:, :nb], in_=banks[sb_i][:, :nb], func=AF.Exp,
                                                            scale=sc, bias=bias),
                              reads=[R_bank[sb_i], R_par], writes=[R_PT[pt_i]])

                    def emit_PV(idx):
                        gi, kt = steps[idx]
                        kind, h, cc = groups[gi]
                        first = kt == ktiles[0]
                        lastk = kt == ktiles[-1]
                        if first:
                            obank_of[gi] = obanks[gi % 3]
                        ob = obank_of[gi]
                        if kind == "A":
                            kv = h // 4
                            blk = (2 * kv + 1) if h % 2 == 0 else (2 * kv)
                        else:
                            blk = (5, 6, 8, 9)[h]
                        lhs = Vb[:, kt, blk:blk + 2, :].rearrange("p b d -> p (b d)")
                        pt_i = idx % 4
                        P.add("pe", lambda e: e.matmul(banks[ob][:, :nb], lhsT=lhs, rhs=PT[pt_i][:, :nb], start=first, stop=lastk),
                              reads=[R_kv, R_PT[pt_i]], writes=[R_bank[ob]])
                        if lastk:
                            finalize(gi)

                    def finalize(gi):
                        kind, h, cc = groups[gi]
                        ob = obank_of[gi]
                        par = h % 2
                        orow, lrow = par * 64, (1 - par) * 64
                        ri = rl_cnt[0] % 3
                        rl_cnt[0] += 1
                        P.add("dve", lambda e: e.reciprocal(out=rl[ri][orow:orow + 64, :nb], in_=banks[ob][lrow:lrow + 64, :nb]),
                              reads=[R_bank[ob]], writes=[R_rl[ri]])
                        if kind == "A":
                            P.add("dve", lambda e: e.tensor_tensor(out=ybc[orow:orow + 64, h // 2, :nb], in0=banks[ob][orow:orow + 64, :nb],
                                                                   in1=rl[ri][orow:orow + 64, :nb], op=ALU.mult),
                                  reads=[R_bank[ob], R_rl[ri]], writes=[R_ybc])
                            return
                        P.add("dve", lambda e: e.tensor_tensor(out=dt_[cc][orow:orow + 64, :nb], in0=banks[ob][orow:orow + 64, :nb],
                                                               in1=rl[ri][orow:orow + 64, :nb], op=ALU.mult),
                              reads=[R_bank[ob], R_rl[ri]], writes=[R_dt[cc]])
                        if cc == 1:
                            P.add("dve", lambda e: e.scalar_tensor_tensor(
                                out=dbuf[orow:orow + 64, h // 2, :nb], in0=dt_[1][orow:orow + 64, :nb],
                                scalar=neglam[orow:orow + 64, l:l + 1], in1=dt_[0][orow:orow + 64, :nb],
                                op0=ALU.mult, op1=ALU.add), reads=[R_dt[0], R_dt[1], R_par], writes=[R_dbuf])
                            if h % 2 == 1:
                                subln(h // 2, ob)

                    def subln(c, bi):
                        P.add("act", lambda e: e.activation(out=dt_[1][:, :nb], in_=dbuf[:, c, :nb], func=AF.Square),
                              reads=[R_dbuf], writes=[R_dt[1]])
                        P.add("pe", lambda e: e.matmul(banks[bi][:, :nb], lhsT=constf[:, 128:256], rhs=dt_[1][:, :nb], start=True, stop=True),
                              reads=[R_dt[1], R_constf], writes=[R_bank[bi]])
                        P.add("act", lambda e: e.activation(out=dt_[0][:, :nb], in_=banks[bi][:, :nb], func=AF.Ln, scale=1.0 / 64, bias=EPS),
                              reads=[R_bank[bi]], writes=[R_dt[0]])
                        P.add("act", lambda e: e.activation(out=dt_[0][:, :nb], in_=dt_[0][:, :nb], func=AF.Exp, scale=-0.5),
                              reads=[R_dt[0]], writes=[R_dt[0]])
                        P.add("dve", lambda e: e.scalar_tensor_tensor(
                            out=ybc[:, 6 + c, :nb], in0=dbuf[:, c, :nb], scalar=subws[:, l:l + 1], in1=dt_[0][:, :nb],
                            op0=ALU.mult, op1=ALU.mult), reads=[R_dbuf, R_dt[0], R_par], writes=[R_ybc])

                    LOOK = 2
                    n = len(steps)
                    for idx in range(min(LOOK, n)):
                        emit_S(idx)
                    for idx in range(n):
                        if idx + LOOK < n:
                            emit_S(idx + LOOK)
                        emit_PV(idx)
                        pump()
                    while pending:
                        pump()
                    tap("yb", ybc[:].rearrange("p k t -> p (k t)"), R_ybc, BF16)

                run(prologue(0))
                for b in range(nblocks):
                    pend = []
                    if b >= 1:
                        pend.append(epilogue(b - 1))
                    if b + 1 < nblocks:
                        pend.append(prologue(b + 1))
                    attention(b, pend)
                run(epilogue(nblocks - 1))
                barrier()

        def ffn(s, l, last, final):
            moe = (l % 2 == 1)
            li = l // 2
            HS = 3
            slabs = [(0, 3), (3, 3), (6, 3), (9, 3), (12, 3), (15, 3), (18, 2), (20, 2)]
            nblocks = 4 if last else 5
            with ExitStack() as st:
                facc = SB(st, "facc", [128, 8, NT], F32)
                h2 = SB(st, "h2", [128, 8, NT], BF16)
                wgu = [SB(st, f"wgu{i}", [128, 8, 2, HS * 128], BF16) for i in range(2)]
                wd = [SB(st, f"wd{i}", [128, HS, D], BF16) for i in range(2)]
                act = [SB(st, f"act{i}", [128, HS, 512], BF16) for i in range(2)]
                sg = [SB(st, f"sg{i}", [128, 512], F32) for i in range(2)]
                tt = [SB(st, f"tt{i}", [128, 512], F32) for i in range(2)]
                sqb = [SB(st, f"fsqb{i}", [128, 512], BF16) for i in range(2)]
                rs = SB(st, "frs", [128, 512], F32)
                tn = [SB(st, f"ftn{i}", [128, 512], F32) for i in range(2)]
                R_facc = [Res() for _ in range(5)]
                R_h2 = [Res() for _ in range(5)]
                R_wgu = [Res(), Res()]
                R_wd = [Res(), Res()]
                R_act = [Res(), Res()]
                R_sg = [Res(), Res()]
                R_tt = [Res(), Res()]
                R_sq = [Res(), Res()]
                R_tn = [Res(), Res()]
                R_rs = Res()
                nbufs = (sqb, R_sq, rs, R_rs, tn, R_tn, 6)
                if moe:
                    comb = SB(st, "comb", [128, NTILE, 8], F32)
                    lg = SB(st, "lg", [128, 4, 8], F32)
                    mx = SB(st, "mx", [128, 8], F32)
                    gt = SB(st, "gt", [128, 8], F32)
                    cc1 = SB(st, "cc1", [128, 8], F32)
                    cc2 = SB(st, "cc2", [128, 8], F32)
                    cwb = [SB(st, f"cwb{i}", [128, NT], BF16) for i in range(2)]
                    R_comb, R_lg, R_rt = Res(), Res(), Res()
                    R_cwb = [Res(), Res()]

                if moe:
                    wlist = [(mwg_d[li, e], mwu_d[li, e], mwd_d[li, e], e, h0, hs) for e in range(NE) for (h0, hs) in slabs]
                else:
                    wlist = [(dwg_d[li], dwu_d[li], dwd_d[li], 0, h0, hs) for (h0, hs) in slabs]

                def load_slab(i):
                    g_d, u_d, d_d, e_, h0, hs = wlist[i]
                    a = i % 2
                    gv = g_d.rearrange("(k p) n -> p k n", p=128)
                    uv = u_d.rearrange("(k p) n -> p k n", p=128)
                    dv = d_d.rearrange("(k p) n -> p k n", p=128)
                    P.add("pool", lambda e: e.dma_start(out=wgu[a][:, :, 0, :hs * 128], in_=gv[:, :, h0 * 128:(h0 + hs) * 128]),
                          writes=[R_wgu[a]], dma=True)
                    P.add("pool", lambda e: e.dma_start(out=wgu[a][:, :, 1, :hs * 128], in_=uv[:, :, h0 * 128:(h0 + hs) * 128]),
                          writes=[R_wgu[a]], dma=True)
                    P.add("pool", lambda e: e.dma_start(out=wd[a][:, :hs, :], in_=dv[:, h0:h0 + hs, :]),
                          writes=[R_wd[a]], dma=True)

                load_slab(0)
                if len(wlist) > 1:
                    load_slab(1)

                for b in range(5 if not last else 4):
                    t_0, nb = BLOCKS[b]
                    P.add("sp", lambda e, t_0=t_0, nb=nb: e.dma_start(out=facc[:, :, t_0:t_0 + nb], in_=xscr[s][:, :, t_0:t_0 + nb]),
                          reads=[R_xscr[s]], writes=[R_facc[b]], dma=True)
                for b in range(nblocks):
                    t_0, nb = BLOCKS[b]
                    bsel = s if b < 4 else 2
                    if not moe:
                        run(norm_block(nbufs, lambda k, t_0=t_0, nb=nb: facc[:, k, t_0:t_0 + nb], R_facc[b], nb, l, 1, bsel,
                                       lambda k, t_0=t_0, nb=nb: h2[:, k, t_0:t_0 + nb], R_h2[b]))
                        continue
                    ro, _ = _off["rw"]

                    def h32cb(k, ap32, R32, nb=nb, li=li):
                        for j in range(nb // 128):
                            P.add("pe", lambda e, j=j: e.matmul(
                                banks[7][:, j * 8:(j + 1) * 8], lhsT=ap32[:, j * 128:(j + 1) * 128],
                                rhs=small[:, ro + li * 64 + k * 8:ro + li * 64 + k * 8 + 8], start=(k == 0 and j == 0), stop=(k == 7),
                                skip_group_check=True),
                                reads=[R32, R_small], writes=[R_bank[7]])
                    run(norm_block(nbufs, lambda k, t_0=t_0, nb=nb: facc[:, k, t_0:t_0 + nb], R_facc[b], nb, l, 1, bsel,
                                   lambda k, t_0=t_0, nb=nb: h2[:, k, t_0:t_0 + nb], R_h2[b], h32=h32cb))
                    nt_ = nb // 128
                    rbo, _ = _off["rb"]
                    P.add("dve", lambda e, nt_=nt_, li=li: e.tensor_tensor(
                        out=lg[:, :nt_, :], in0=banks[7][:, 0:nt_ * 8].rearrange("p (j e) -> p j e", e=8),
                        in1=small[:, rbo + li * 8:rbo + li * 8 + 8].unsqueeze(1).to_broadcast([128, nt_, 8]), op=ALU.add),
                        reads=[R_bank[7], R_small], writes=[R_lg])
                    for j in range(nt_):
                        ti = t_0 // 128 + j
                        P.add("dve", lambda e, j=j: e.max(out=mx[:], in_=lg[:, j, :]), reads=[R_lg], writes=[R_rt])
                        P.add("dve", lambda e: e.tensor_tensor(out=gt[:, 0:1], in0=mx[:, 1:2], in1=mx[:, 0:1], op=ALU.subtract),
                              reads=[R_rt], writes=[R_rt])
                        P.add("act", lambda e: e.activation(out=gt[:, 1:2], in_=gt[:, 0:1], func=AF.Exp), reads=[R_rt], writes=[R_rt])
                        P.add("dve", lambda e: e.tensor_scalar_add(out=gt[:, 2:3], in0=gt[:, 1:2], scalar1=1.0), reads=[R_rt], writes=[R_rt])
                        P.add("dve", lambda e: e.reciprocal(out=gt[:, 3:4], in_=gt[:, 2:3]), reads=[R_rt], writes=[R_rt])
                        P.add("dve", lambda e: e.tensor_tensor(out=gt[:, 4:5], in0=gt[:, 1:2], in1=gt[:, 3:4], op=ALU.mult),
                              reads=[R_rt], writes=[R_rt])
                        P.add("dve", lambda e, j=j: e.tensor_scalar(out=cc1[:], in0=lg[:, j, :], scalar1=mx[:, 0:1], scalar2=gt[:, 3:4],
                                                                    op0=ALU.is_equal, op1=ALU.mult), reads=[R_lg, R_rt], writes=[R_rt])
                        P.add("dve", lambda e, j=j: e.tensor_scalar(out=cc2[:], in0=lg[:, j, :], scalar1=mx[:, 1:2], scalar2=gt[:, 4:5],
                                                                    op0=ALU.is_equal, op1=ALU.mult), reads=[R_lg, R_rt], writes=[R_rt])
                        P.add("dve", lambda e, ti=ti: e.tensor_tensor(out=comb[:, ti, :], in0=cc1[:], in1=cc2[:], op=ALU.add),
                              reads=[R_rt], writes=[R_comb])

                if moe:
                    tap("comb", comb[:].rearrange("p t e -> p (t e)"), R_comb)
                    tap("lg", lg[:].rearrange("p t e -> p (t e)"), R_lg)
                    tap("h2m", h2[:, :, 0:512], R_h2[0], BF16)
                tap("h2", h2[:, :, 0:512], R_h2[0], BF16)
                tap("facc0", facc[:, :, 0:512], R_facc[0])
                units = [(i, b) for i in range(len(wlist)) for b in range(nblocks)]
                gcnt = [0]
                ocnt = [0]
                cur_e = [-1]

                def make_cwb(e_):
                    a = e_ % 2
                    for b in range(nblocks):
                        t_0, nb = BLOCKS[b]
                        for j in range(nb // 128):
                            ti = t_0 // 128 + j
                            P.add("pe", lambda e, ti=ti, j=j: e.matmul(
                                banks[7][:, j * 128:(j + 1) * 128], lhsT=comb[:, ti, e_:e_ + 1].to_broadcast([128, 128]),
                                rhs=identf, start=True, stop=True), reads=[R_comb, R_constf], writes=[R_bank[7]])
                        P.add("act", lambda e, t_0=t_0, nb=nb: e.activation(out=cwb[a][:, t_0:t_0 + nb], in_=banks[7][:, :nb], func=AF.Copy),
                              reads=[R_bank[7]], writes=[R_cwb[a]])

                def emit_GU(u):
                    i, b = units[u]
                    _, _, _, e_, h0, hs = wlist[i]
                    if moe and e_ != cur_e[0]:
                        cur_e[0] = e_
                        make_cwb(e_)
                    a = i % 2
                    t_0, nb = BLOCKS[b]
                    aa = u % 2
                    for hc in range(hs):
                        gi = gcnt[0] % 2
                        gcnt[0] += 1
                        gb, ub = gi, 2 + gi
                        for k in range(8):
                            P.add("pe", lambda e, k=k, hc=hc, gb=gb: e.matmul(
                                banks[gb][:, :nb], lhsT=wgu[a][:, k, 0, hc * 128:(hc + 1) * 128], rhs=h2[:, k, t_0:t_0 + nb],
                                start=(k == 0), stop=(k == 7)), reads=[R_wgu[a], R_h2[b]], writes=[R_bank[gb]])
                        for k in range(8):
                            P.add("pe", lambda e, k=k, hc=hc, ub=ub: e.matmul(
                                banks[ub][:, :nb], lhsT=wgu[a][:, k, 1, hc * 128:(hc + 1) * 128], rhs=h2[:, k, t_0:t_0 + nb],
                                start=(k == 0), stop=(k == 7)), reads=[R_wgu[a], R_h2[b]], writes=[R_bank[ub]])
                        P.add("act", lambda e, gi=gi, gb=gb: e.activation(out=sg[gi][:, :nb], in_=banks[gb][:, :nb], func=AF.Silu),
                              reads=[R_bank[gb]], writes=[R_sg[gi]])
                        if moe:
                            P.add("dve", lambda e, gi=gi, ub=ub: e.tensor_tensor(out=tt[gi][:, :nb], in0=banks[ub][:, :nb], in1=sg[gi][:, :nb],
                                                                                 op=ALU.mult), reads=[R_bank[ub], R_sg[gi]], writes=[R_tt[gi]])
                            P.add("dve", lambda e, gi=gi, hc=hc: e.tensor_tensor(
                                out=act[aa][:, hc, :nb], in0=tt[gi][:, :nb], in1=cwb[e_ % 2][:, t_0:t_0 + nb], op=ALU.mult),
                                reads=[R_tt[gi], R_cwb[e_ % 2]], writes=[R_act[aa]])
                        else:
                            P.add("dve", lambda e, gi=gi, ub=ub, hc=hc: e.tensor_tensor(
                                out=act[aa][:, hc, :nb], in0=banks[ub][:, :nb], in1=sg[gi][:, :nb], op=ALU.mult),
                                reads=[R_bank[ub], R_sg[gi]], writes=[R_act[aa]])

                def emit_D(u):
                    i, b = units[u]
                    _, _, _, e_, h0, hs = wlist[i]
                    a = i % 2
                    t_0, nb = BLOCKS[b]
                    aa = u % 2
                    bsel = s if b < 4 else 2
                    for oc in range(8):
                        ob = 4 + ocnt[0] % 2
                        ocnt[0] += 1
                        for hc in range(hs):
                            P.add("pe", lambda e, hc=hc, oc=oc, ob=ob: e.matmul(
                                banks[ob][:, :nb], lhsT=wd[a][:, hc, oc * 128:(oc + 1) * 128], rhs=act[aa][:, hc, :nb],
                                start=(hc == 0), stop=(hc == hs - 1)), reads=[R_wd[a], R_act[aa]], writes=[R_bank[ob]])
                        P.add("dve", lambda e, oc=oc, ob=ob: e.scalar_tensor_tensor(
                            out=facc[:, oc, t_0:t_0 + nb], in0=banks[ob][:, :nb], scalar=modT[:, l, 40 + oc, bsel:bsel + 1],
                            in1=facc[:, oc, t_0:t_0 + nb], op0=ALU.mult, op1=ALU.add),
                            reads=[R_bank[ob], R_facc[b], R_modT], writes=[R_facc[b]])
                    if b == nblocks - 1 and i + 2 < len(wlist):
                        load_slab(i + 2)

                nu = len(units)
                emit_GU(0)
                for u in range(nu):
                    if u + 1 < nu:
                        emit_GU(u + 1)
                    emit_D(u)

                if final:
                    yt = [SB(st, f"yt{i}", [128, D], F32) for i in range(1)]
                    R_yt = [Res()]
                    for i in range(16):
                        a = 0
                        b = i // 4
                        for k in range(8):
                            bi = k // 4
                            P.add("pe", lambda e, i=i, k=k, bi=bi: e.transpose(
                                banks[bi][:, (k % 4) * 128:(k % 4 + 1) * 128], facc[:, k, i * 128:(i + 1) * 128], identf),
                                reads=[R_facc[b], R_constf], writes=[R_bank[bi]])
                        P.add("act", lambda e, a=a: e.activation(out=yt[a][:, 0:512], in_=banks[0][:], func=AF.Copy),
                              reads=[R_bank[0]], writes=[R_yt[a]])
                        P.add("dve", lambda e, a=a: e.tensor_copy(out=yt[a][:, 512:1024], in_=banks[1][:]),
                              reads=[R_bank[1]], writes=[R_yt[a]])
                        od = P.add("sp", lambda e, a=a, i=i: e.dma_start(out=y_d[s, i * 128:(i + 1) * 128, :], in_=yt[a][:]),
                                   reads=[R_yt[a]], dma=True)
                        out_dmas.append(od)
                else:
                    for b in range(nblocks):
                        t_0, nb = BLOCKS[b]
                        P.add("sp", lambda e, t_0=t_0, nb=nb: e.dma_start(out=xscr[s][:, :, t_0:t_0 + nb], in_=facc[:, :, t_0:t_0 + nb]),
                              reads=[R_facc[b]], writes=[R_xscr[s]], dma=True)
                barrier()

        for s in seqs:
            ingest(s)
            for l in range(n_layers):
                last = (l == n_layers - 1)
                mixer(s, l, last)
                ffn(s, l, last, final=last)

        P.emit(nc, final_waits=out_dmas)
    return nc


def pack_small(core, inp):
    f = lambda a: np.asarray(a, dtype=np.float32)
    sm = np.zeros((128, NS), np.float32)

    def put(name, arr):
        o, w = _off[name]
        arr = np.ascontiguousarray(arr, dtype=np.float32).reshape(128, w)
        sm[:, o:o + w] = arr

    c = f(inp["c"])
    cv = np.stack([c[2 * core], c[2 * core + 1], f(inp["c_ctx"])], axis=0)
    put("cT", cv.reshape(3, 8, 128).transpose(2, 1, 0))
    put("b_ada", f(inp["b_ada"]).reshape(4, 48, 128).transpose(2, 0, 1))
    put("nmix", f(inp["norm_mix"]).reshape(4, 8, 128).transpose(2, 0, 1))
    put("nffn", f(inp["norm_ffn"]).reshape(4, 8, 128).transpose(2, 0, 1))
    put("gqA", np.tile(f(inp["a_q_gain"]), (1, 2)).T)
    put("gkA", np.tile(f(inp["a_k_gain"]), (1, 2)).T)
    put("gqD", np.tile(f(inp["d_q_gain"]), (1, 4)).T)
    put("gkD", np.tile(f(inp["d_k_gain"]), (1, 4)).T)
    put("subw", np.tile(f(inp["diff_subln"]), (1, 2)).T)
    put("conv", f(inp["conv_w"]).reshape(4, 3, 2, 128).transpose(3, 0, 1, 2))
    put("lam", np.broadcast_to(f(inp["diff_lambda"]).reshape(1, 512), (128, 512)))
    put("rb", np.broadcast_to(f(inp["router_b"]).reshape(1, 16), (128, 16)))
    put("rw", f(inp["router_w"]).reshape(2, 8, 128, 8).transpose(2, 0, 1, 3))
    put("aq_rep", np.broadcast_to(f(inp["a_q_gain"]).reshape(1, 256), (128, 256)))
    put("ak_rep", np.broadcast_to(f(inp["a_k_gain"]).reshape(1, 256), (128, 256)))
    put("dq_rep", np.broadcast_to(f(inp["d_q_gain"]).reshape(1, 128), (128, 128)))
    put("dk_rep", np.broadcast_to(f(inp["d_k_gain"]).reshape(1, 128), (128, 128)))
    return sm


_SHARED = ("w_ada", "w_in", "w_out", "dense_w_gate", "dense_w_up", "dense_w_down",
           "moe_w_gate", "moe_w_up", "moe_w_down")


def make_in_maps(inp, cores):
    consts = make_consts()
    rope = make_rope()
    shared = {k: np.ascontiguousarray(np.asarray(inp[k], dtype=np.float32)) for k in _SHARED}
    x = np.asarray(inp["x"], dtype=np.float32)
    ctx = np.asarray(inp["ctx"], dtype=np.float32)
    maps = []
    for core in cores:
        m = dict(shared)
        m["x"] = np.ascontiguousarray(x[2 * core:2 * core + 2])
        m["ctx"] = np.ascontiguousarray(ctx[2 * core:2 * core + 2])
        m["small"] = pack_small(core, inp)
        m["consts"] = consts
        m["rope"] = rope
        maps.append(m)
    return maps


def kernel(**inputs):
    nc = build_program()
    in_maps = make_in_maps(inputs, list(range(8)))
    res = run_bass_kernel_spmd(nc, in_maps, core_ids=list(range(8)))
    out = np.concatenate([np.asarray(r["y"], dtype=np.float32) for r in res.results], axis=0)
    return out
```

```python
import math
from contextlib import ExitStack

import numpy as np
import concourse.bass as bass
import concourse.mybir as mybir
from concourse.bass_utils import run_bass_kernel_spmd

F32 = mybir.dt.float32
BF16 = mybir.dt.bfloat16
ALU = mybir.AluOpType
AF = mybir.ActivationFunctionType
AX = mybir.AxisListType

ENGS = ("pe", "act", "dve", "pool", "sp")
SEM_CAP = 30000
GAP = 2
N_DMA_SEMS = 12


class Res:
    __slots__ = ("w", "r", "name")

    def __init__(self, name=""):
        self.w = {}
        self.r = {}
        self.name = name


class Op:
    __slots__ = ("eng", "fn", "deps", "dma", "sig", "nsig", "slot", "slotval", "key")

    def __init__(self, eng, fn, dma):
        self.eng = eng
        self.fn = fn
        self.dma = dma
        self.deps = []
        self.sig = False
        self.nsig = 0
        self.slot = 0
        self.slotval = 0


class Prog:
    def __init__(self):
        self.ops = {e: [] for e in ENGS}
        self.uid = 0
        self.dmas_since_barrier = []

    def add(self, eng, fn, reads=(), writes=(), dma=False, extra_deps=()):
        op = Op(eng, fn, dma)
        self.uid += 1
        key = ("d", self.uid) if dma else eng
        op.key = key
        deps = {}

        def dep(o, raw):
            if o is op:
                return
            if (not dma) and (not o.dma) and o.eng == eng:
                if eng == "pe":
                    return
            deps[id(o)] = o

        for r in reads:
            for o in r.w.values():
                dep(o, True)
        for w in writes:
            for o in w.r.values():
                dep(o, False)
            for o in w.w.values():
                dep(o, False)
        for o in extra_deps:
            dep(o, True)
        for r in reads:
            r.r[key] = op
        for w in writes:
            if w.r:
                w.w = {key: op}
                w.r = {}
            else:
                w.w[key] = op
        op.deps = list(deps.values())
        for o in op.deps:
            o.sig = True
        self.ops[eng].append(op)
        if dma:
            self.dmas_since_barrier.append(op)
        return op

    def emit(self, nc, final_waits=()):
        nsem = {}
        for e in ENGS:
            c = 0
            for op in self.ops[e]:
                if op.dma:
                    continue
                if op.sig:
                    c += 1
                    op.nsig = c
            nsem[e] = max(1, (c + SEM_CAP - 1) // SEM_CAP)
        for e in ENGS:
            k = 0
            for op in self.ops[e]:
                if op.dma:
                    op.slot = k % N_DMA_SEMS
                    op.slotval = 16 * (k // N_DMA_SEMS + 1)
                    k += 1
        with ExitStack() as st:
            sems = {e: [st.enter_context(nc.semaphore(f"s_{e}_{i}")) for i in range(nsem[e])]
                    for e in ENGS}
            dsems = {e: [st.enter_context(nc.semaphore(f"d_{e}_{i}")) for i in range(N_DMA_SEMS)]
                     for e in ENGS if any(o.dma for o in self.ops[e])}
            block = st.enter_context(nc.Block())

            def target(o):
                if o.dma:
                    return (("d", o.eng, o.slot), dsems[o.eng][o.slot], o.slotval)
                i = (o.nsig - 1) // SEM_CAP
                return (("s", o.eng, i), sems[o.eng][i], o.nsig - i * SEM_CAP)

            def run(e, eng):
                waited = {}
                last_in_slot = {}
                for op in self.ops[e]:
                    tg = [target(o) for o in op.deps]
                    if op.dma and op.slot in last_in_slot:
                        tg.append(target(last_in_slot[op.slot]))
                    best = {}
                    for k, s, v in tg:
                        if waited.get(k, 0) >= v:
                            continue
                        if k not in best or best[k][1] < v:
                            best[k] = (s, v)
                    for k, (s, v) in best.items():
                        eng.wait_ge(s, v)
                        waited[k] = v
                    ins = op.fn(eng)
                    if op.dma:
                        ins.then_inc(dsems[e][op.slot], 16)
                        last_in_slot[op.slot] = op
                    elif op.sig:
                        i = (op.nsig - 1) // SEM_CAP
                        ins.then_inc(sems[e][i], 1)
                for o in final_waits:
                    if o.eng == e:
                        k, s, v = target(o)
                        eng.wait_ge(s, v)

            block.tensor(lambda eng: run("pe", eng))
            block.scalar(lambda eng: run("act", eng))
            block.vector(lambda eng: run("dve", eng))
            block.gpsimd(lambda eng: run("pool", eng))
            block.sync(lambda eng: run("sp", eng))


D = 1024
NLAT = 2048
NCTX = 256
NT = NLAT + NCTX
DEPTH = 4
DFF = 2816
NE = 8
EPS = 1e-6
BLOCKS = [(0, 512), (512, 512), (1024, 512), (1536, 512), (2048, 256)]
NTILE = NT // 128

_off = {}
_c = 0
for _n, _w in (("cT", 24), ("b_ada", 192), ("nmix", 32), ("nffn", 32), ("gqA", 4), ("gkA", 4),
               ("gqD", 4), ("gkD", 4), ("subw", 4), ("conv", 24), ("lam", 512), ("rb", 16),
               ("rw", 128), ("aq_rep", 256), ("ak_rep", 256), ("dq_rep", 128), ("dk_rep", 128)):
    _off[_n] = (_c, _w)
    _c += _w
NS = _c
NCONST = 768


def lam_init_of(l):
    return 0.8 - 0.6 * math.exp(-0.3 * l)


def make_consts():
    c = np.zeros((128, NCONST), np.float32)
    c[:, 0:128] = 1.0
    for p in range(128):
        for m in range(128):
            if p // 64 == m // 64:
                c[p, 128 + m] = 1.0
            if p // 32 == m // 32:
                c[p, 256 + m] = 1.0
    for m in range(128):
        d = m % 32
        if d < 16:
            c[m + 16, 384 + m] = -1.0
        else:
            c[m - 16, 384 + m] = 1.0
        d = m % 16
        if d < 8:
            c[m + 8, 512 + m] = -1.0
        else:
            c[m - 8, 512 + m] = 1.0
    c[:, 640:768] = np.eye(128, dtype=np.float32)
    return c


def make_rope():
    t = np.arange(NLAT)
    row = (t // 64).astype(np.float64)
    col = (t % 64).astype(np.float64)
    out = np.zeros((4, 128, NLAT), np.float64)
    for p in range(128):
        d = p % 64
        pos = row if d < 32 else col
        i = d % 16
        inv = 10000.0 ** (-(2.0 * i) / 32.0)
        out[0, p] = np.cos(pos * inv)
        out[1, p] = np.sin(pos * inv)
        d = p % 32
        pos = row if d < 16 else col
        i = d % 8
        inv = 10000.0 ** (-(2.0 * i) / 16.0)
        out[2, p] = np.cos(pos * inv)
        out[3, p] = np.sin(pos * inv)
    return out.astype(np.float32)


def build_program(n_layers=DEPTH, seqs=(0, 1), debug=False):
    nc = bass.Bass("TRN2", target_bir_lowering=False)

    def din(name, shape):
        return nc.dram_tensor(name, list(shape), F32, kind="ExternalInput").ap()

    x_d = din("x", [2, NLAT, D])
    ctx_d = din("ctx", [2, NCTX, D])
    small_d = din("small", [128, NS])
    const_d = din("consts", [128, NCONST])
    rope_d = din("rope", [4, 128, NLAT])
    w_ada_d = din("w_ada", [DEPTH, D, 6 * D])
    w_in_d = din("w_in", [DEPTH, D, 2304])
    w_out_d = din("w_out", [DEPTH, D, D])
    dwg_d = din("dense_w_gate", [2, D, DFF])
    dwu_d = din("dense_w_up", [2, D, DFF])
    dwd_d = din("dense_w_down", [2, DFF, D])
    mwg_d = din("moe_w_gate", [2, NE, D, DFF])
    mwu_d = din("moe_w_up", [2, NE, D, DFF])
    mwd_d = din("moe_w_down", [2, NE, DFF, D])
    y_d = nc.dram_tensor("y", [2, NLAT, D], F32, kind="ExternalOutput").ap()
    xscr = nc.dram_tensor("xscr", [2, 128, 8, NT], F32, kind=("ExternalOutput" if debug else "Internal")).ap()

    P = Prog()
    R_xscr = [Res("xscr0"), Res("xscr1")]
    out_dmas = []
    tapped = set()

    def tap(name, ap, R, dt=F32):
        if not debug or name in tapped:
            return
        tapped.add(name)
        dd = nc.dram_tensor("dbg_" + name, list(ap.shape), dt, kind="ExternalOutput").ap()
        out_dmas.append(P.add("sp", lambda e: e.dma_start(out=dd, in_=ap), reads=[R], dma=True))

    with ExitStack() as top:
        sbcnt = [0]

        def SB(st, name, shape, dt):
            sbcnt[0] += 1
            return st.enter_context(nc.sbuf_tensor(f"sb{sbcnt[0]}_{name}", list(shape), dt))

        small = SB(top, "small", [128, NS], F32)
        constf = SB(top, "constf", [128, NCONST], F32)
        constb = SB(top, "constb", [128, 640], BF16)
        sT = SB(top, "sT", [128, 8, 3], F32)
        modT = SB(top, "modT", [128, DEPTH, 48, 3], F32)
        AA = SB(top, "AA", [128, DEPTH, 2, 8, 3], F32)
        neglam = SB(top, "neglam", [128, DEPTH], F32)
        nbA = SB(top, "nbA", [128, DEPTH], F32)
        nbD = SB(top, "nbD", [128, DEPTH], F32)
        subws = SB(top, "subws", [128, DEPTH], F32)
        misc = SB(top, "misc", [128, 64], F32)
        bdummy = SB(top, "bdummy", [128, 8], F32)
        banks = [top.enter_context(nc.psum_tensor(f"bank{i}", [128, 512], F32)) for i in range(8)]
        R_bank = [Res(f"bank{i}") for i in range(8)]
        R_small, R_constf, R_constb, R_sT, R_modT, R_par = (Res() for _ in range(6))
        R_bar = {e: Res("bar_" + e) for e in ENGS}

        def sm(name, lo=0, hi=None):
            o, w = _off[name]
            hi = w if hi is None else hi
            return small[:, o + lo:o + hi]

        ones_b = constb[:, 0:128]
        bd64_b = constb[:, 128:256]
        bd32_b = constb[:, 256:384]
        rotA_b = constb[:, 384:512]
        rotD_b = constb[:, 512:640]
        identf = constf[:, 640:768]

        def barrier():
            dl = list(P.dmas_since_barrier)
            P.dmas_since_barrier = []
            P.add("pe", lambda e: e.matmul(banks[7][0:1, 0:1], lhsT=constb[0:1, 0:1], rhs=constb[0:1, 0:1],
                                           start=True, stop=True), reads=[R_constb], writes=[R_bar["pe"], R_bank[7]])
            P.add("act", lambda e: e.activation(out=bdummy[0:1, 0:1], in_=bdummy[0:1, 0:1], func=AF.Copy),
                  writes=[R_bar["act"]])
            P.add("dve", lambda e: e.tensor_copy(out=bdummy[0:1, 1:2], in_=bdummy[0:1, 1:2]), writes=[R_bar["dve"]])
            P.add("pool", lambda e: e.memset(bdummy[0:1, 2:3], 0.0), writes=[R_bar["pool"]])
            P.add("sp", lambda e: e.dma_start(out=bdummy[0:1, 4:8], in_=const_d[0:1, 0:4]),
                  writes=[R_bar["sp"]], dma=True, extra_deps=dl)
            allb = list(R_bar.values())
            P.add("pe", lambda e: e.matmul(banks[7][0:1, 0:1], lhsT=constb[0:1, 0:1], rhs=constb[0:1, 0:1],
                                           start=True, stop=True), reads=allb + [R_constb], writes=[R_bank[7]])
            P.add("act", lambda e: e.activation(out=bdummy[0:1, 0:1], in_=bdummy[0:1, 0:1], func=AF.Copy), reads=allb)
            P.add("dve", lambda e: e.tensor_copy(out=bdummy[0:1, 1:2], in_=bdummy[0:1, 1:2]), reads=allb)
            P.add("pool", lambda e: e.memset(bdummy[0:1, 2:3], 0.0), reads=allb)
            P.add("sp", lambda e: e.dma_start(out=bdummy[0:1, 4:8], in_=const_d[0:1, 0:4]), reads=allb, dma=True)
            P.dmas_since_barrier = []

        P.add("sp", lambda e: e.dma_start(out=small[:], in_=small_d), writes=[R_small], dma=True)
        P.add("sp", lambda e: e.dma_start(out=constf[:], in_=const_d), writes=[R_constf], dma=True)
        P.add("dve", lambda e: e.tensor_copy(out=constb[:], in_=constf[:, 0:640]), reads=[R_constf], writes=[R_constb])
        P.add("pool", lambda e: e.memset(bdummy[:], 0.0), writes=list(R_bar.values()))
        P.add("act", lambda e: e.activation(out=sT[:].rearrange("p k j -> p (k j)"), in_=sm("cT"), func=AF.Silu),
              reads=[R_small], writes=[R_sT])

        def pre_params():
            lam4 = sm("lam").rearrange("p (l f d) -> p l f d", l=4, f=4)
            prod = misc[:, 0:16]
            tmp = SB(top, "lamtmp", [128, 4, 2, 32], F32)
            P.add("dve", lambda e: e.tensor_tensor(out=tmp[:], in0=lam4[:, :, 0:4:2, :], in1=lam4[:, :, 1:4:2, :],
                                                   op=ALU.mult), reads=[R_small], writes=[R_par])
            P.add("dve", lambda e: e.tensor_reduce(out=misc[:, 0:8], in_=tmp[:].rearrange("p l f d -> p (l f) d"),
                                                   axis=AX.X, op=ALU.add), reads=[R_par], writes=[R_par])
            P.add("act", lambda e: e.activation(out=misc[:, 8:16], in_=misc[:, 0:8], func=AF.Exp),
                  reads=[R_par], writes=[R_par])
            ev = misc[:, 8:16].rearrange("p (l f) -> p l f", f=2)
            P.add("dve", lambda e: e.tensor_tensor(out=neglam[:], in0=ev[:, :, 1], in1=ev[:, :, 0], op=ALU.subtract),
                  reads=[R_par], writes=[R_par])
            for l in range(DEPTH):
                P.add("dve", lambda e, l=l: e.tensor_scalar_add(out=neglam[:, l:l + 1], in0=neglam[:, l:l + 1],
                                                                scalar1=-lam_init_of(l)), reads=[R_par], writes=[R_par])
                P.add("dve", lambda e, l=l: e.tensor_scalar_mul(out=subws[:, l:l + 1], in0=sm("subw", l, l + 1),
                                                                scalar1=1.0 - lam_init_of(l)),
                      reads=[R_small], writes=[R_par])
            for j, (nm, w) in enumerate((("aq_rep", 64), ("ak_rep", 64), ("dq_rep", 32), ("dk_rep", 32))):
                P.add("dve", lambda e, nm=nm, w=w, j=j: e.reduce_max(
                    out=misc[:, 16 + 4 * j:20 + 4 * j], in_=sm(nm).rearrange("p (l d) -> p l d", d=w),
                    axis=AX.X, apply_absolute_value=True), reads=[R_small], writes=[R_par])
            P.add("dve", lambda e: e.scalar_tensor_tensor(out=nbA[:], in0=misc[:, 16:20], scalar=-8.0, in1=misc[:, 20:24],
                                                          op0=ALU.mult, op1=ALU.mult), reads=[R_par], writes=[R_par])
            P.add("dve", lambda e: e.scalar_tensor_tensor(out=nbD[:], in0=misc[:, 24:28], scalar=-math.sqrt(32.0),
                                                          in1=misc[:, 28:32], op0=ALU.mult, op1=ALU.mult),
                  reads=[R_par], writes=[R_par])

        pre_params()

        with ExitStack() as st:
            wada = [SB(st, f"wada{i}", [128, 8, 768], BF16) for i in range(2)]
            sTb = SB(st, "sTb", [128, 8, 3], BF16)
            P.add("dve", lambda e: e.tensor_copy(out=sTb[:], in_=sT[:]), reads=[R_sT], writes=[R_sT])
            R_wada = [Res(), Res()]
            cnt = 0
            for l in range(n_layers):
                wv = w_ada_d[l].rearrange("(k p) n -> p k n", p=128)
                for g in range(8):
                    a = cnt % 2
                    cnt += 1
                    P.add("pool", lambda e, a=a, wv=wv, g=g: e.dma_start(out=wada[a][:], in_=wv[:, :, g * 768:(g + 1) * 768]),
                          writes=[R_wada[a]], dma=True)
                    bk = banks[g % 2]
                    for jj in range(6):
                        for k in range(8):
                            P.add("pe", lambda e, a=a, jj=jj, k=k, bk=bk: e.matmul(
                                bk[:, jj * 4:jj * 4 + 3], lhsT=wada[a][:, k, jj * 128:(jj + 1) * 128], rhs=sTb[:, k, :],
                                start=(k == 0), stop=(k == 7)), reads=[R_wada[a], R_sT], writes=[R_bank[g % 2]])
                    bo, _ = _off["b_ada"]
                    P.add("dve", lambda e, l=l, g=g, bk=bk, bo=bo: e.tensor_tensor(
                        out=modT[:, l, g * 6:(g + 1) * 6, :],
                        in0=bk[:, 0:24].rearrange("p (j c) -> p j c", c=4)[:, :, 0:3],
                        in1=small[:, bo + l * 48 + g * 6:bo + l * 48 + g * 6 + 6].unsqueeze(2).to_broadcast([128, 6, 3]),
                        op=ALU.add), reads=[R_bank[g % 2], R_small], writes=[R_modT])
                for which, (nm, base) in enumerate((("nmix", 8), ("nffn", 32))):
                    P.add("dve", lambda e, l=l, which=which, nm=nm, base=base: e.scalar_tensor_tensor(
                        out=AA[:, l, which, :, :], in0=modT[:, l, base:base + 8, :], scalar=1.0,
                        in1=sm(nm, l * 8, l * 8 + 8).unsqueeze(2).to_broadcast([128, 8, 3]),
                        op0=ALU.add, op1=ALU.mult), reads=[R_modT, R_small], writes=[R_modT])
            tap("modT", modT[:].rearrange("p l j c -> p (l j c)"), R_modT)
            tap("AA", AA[:].rearrange("p l w k c -> p (l w k c)"), R_modT)
            tap("neglam", neglam[:], R_par)
            tap("nbA", nbA[:], R_par)
            tap("subws", subws[:], R_par)
            barrier()

        def run(gen):
            for _ in gen:
                pass

        def rr(gens):
            gens = list(gens)
            while gens:
                for g in list(gens):
                    try:
                        next(g)
                    except StopIteration:
                        gens.remove(g)

        def norm_block(st_bufs, xget, R_x, nb, l, which, bsel, hout, R_h, h32=None):
            sqb, R_sq, rs, R_rs, tn, R_tn, bank_i = st_bufs
            bk = banks[bank_i]
            def _sq(k):
                a = k % 2
                P.add("act", lambda e: e.activation(out=sqb[a][:, :nb], in_=xget(k), func=AF.Square),
                      reads=[R_x], writes=[R_sq[a]])

            def _mm(k):
                a = k % 2
                P.add("pe", lambda e: e.matmul(bk[:, :nb], lhsT=ones_b, rhs=sqb[a][:, :nb],
                                               start=(k == 0), stop=(k == 7)),
                      reads=[R_sq[a], R_constb], writes=[R_bank[bank_i]])
            _sq(0)
            yield
            _sq(1)
            for _ in range(GAP):
                yield
            for k in range(8):
                _mm(k)
                yield
                if k + 2 < 8:
                    _sq(k + 2)
                    yield
                    yield
            P.add("act", lambda e: e.activation(out=rs[:, :nb], in_=bk[:, :nb], func=AF.Ln, scale=1.0 / D, bias=EPS),
                  reads=[R_bank[bank_i]], writes=[R_rs])
            P.add("act", lambda e: e.activation(out=rs[:, :nb], in_=rs[:, :nb], func=AF.Exp, scale=-0.5),
                  reads=[R_rs], writes=[R_rs])
            shb = 0 if which == 0 else 24
            yield
            for k in range(8):
                a = k % 2
                P.add("dve", lambda e, k=k, a=a: e.tensor_tensor(out=tn[a][:, :nb], in0=xget(k), in1=rs[:, :nb], op=ALU.mult),
                      reads=[R_x, R_rs], writes=[R_tn[a]])
                if h32 is None:
                    P.add("dve", lambda e, k=k, a=a: e.tensor_scalar(
                        out=hout(k), in0=tn[a][:, :nb], scalar1=AA[:, l, which, k, bsel:bsel + 1],
                        scalar2=modT[:, l, shb + k, bsel:bsel + 1], op0=ALU.mult, op1=ALU.add),
                        reads=[R_tn[a], R_modT], writes=[R_h])
                else:
                    P.add("dve", lambda e, k=k, a=a: e.tensor_scalar(
                        out=tn[a][:, :nb], in0=tn[a][:, :nb], scalar1=AA[:, l, which, k, bsel:bsel + 1],
                        scalar2=modT[:, l, shb + k, bsel:bsel + 1], op0=ALU.mult, op1=ALU.add),
                        reads=[R_tn[a], R_modT], writes=[R_tn[a]])
                    P.add("act", lambda e, k=k, a=a: e.activation(out=hout(k), in_=tn[a][:, :nb], func=AF.Copy),
                          reads=[R_tn[a]], writes=[R_h])
                    h32(k, tn[a], R_tn[a])
                yield

        def ingest(s):
            with ExitStack() as st:
                xt = [SB(st, f"ixt{i}", [128, D], F32) for i in range(2)]
                xT = [SB(st, f"ixT{i}", [128, 8, 128], F32) for i in range(2)]
                R_xt = [Res(), Res()]
                R_xT = [Res(), Res()]
                for i in range(NTILE):
                    a = i % 2
                    src = x_d[s, i * 128:(i + 1) * 128, :] if i < 16 else ctx_d[s, (i - 16) * 128:(i - 15) * 128, :]
                    P.add("sp", lambda e, a=a, src=src: e.dma_start(out=xt[a][:], in_=src), writes=[R_xt[a]], dma=True)
                    for k in range(8):
                        bi = k // 4
                        P.add("pe", lambda e, a=a, k=k, bi=bi: e.transpose(
                            banks[bi][:, (k % 4) * 128:(k % 4 + 1) * 128], xt[a][:, k * 128:(k + 1) * 128], identf),
                            reads=[R_xt[a], R_constf], writes=[R_bank[bi]])
                    P.add("act", lambda e, a=a: e.activation(out=xT[a][:, 0:4, :].rearrange("p k t -> p (k t)"),
                                                             in_=banks[0][:], func=AF.Copy),
                          reads=[R_bank[0]], writes=[R_xT[a]])
                    P.add("dve", lambda e, a=a: e.tensor_copy(out=xT[a][:, 4:8, :].rearrange("p k t -> p (k t)"),
                                                              in_=banks[1][:]),
                          reads=[R_bank[1]], writes=[R_xT[a]])
                    P.add("sp", lambda e, a=a, i=i: e.dma_start(out=xscr[s][:, :, i * 128:(i + 1) * 128], in_=xT[a][:]),
                          reads=[R_xT[a]], writes=[R_xscr[s]], dma=True)
                barrier()

        def mixer(s, l, last):
            with ExitStack() as st:
                wA = SB(st, "wA", [128, 8, 2048], BF16)
                w1 = wA
                w2 = wA[:, :, 0:1024]
                wo = wA[:, :, 1024:2048]
                kA = SB(st, "kA", [128, 2, NT], BF16)
                kD = SB(st, "kD", [128, 2, NT], BF16)
                zb = SB(st, "zb", [128, 2, NT + 4], BF16)
                Vb = SB(st, "Vb", [128, NTILE, 11, 64], BF16)
                xb = [SB(st, f"xb{i}", [128, 8, 512], F32) for i in range(1)]
                hb = [SB(st, f"hb{i}", [128, 8, 512], BF16) for i in range(1)]
                tabs = [SB(st, f"tabs{i}", [128, 4, 512], F32) for i in range(1)]
                q6 = [SB(st, f"q6{i}", [128, 6, 512], BF16) for i in range(2)]
                yb = [SB(st, f"yb{i}", [128, 8, 512], BF16) for i in range(2)]
                PT = [SB(st, f"PT{i}", [128, 512], BF16) for i in range(4)]
                sqb = [SB(st, f"sqb{i}", [128, 512], BF16) for i in range(2)]
                rs = SB(st, "rs", [128, 512], F32)
                tn = [SB(st, f"tn{i}", [128, 512], F32) for i in range(2)]
                t0 = [SB(st, f"t0{i}", [128, 512], F32) for i in range(2)]
                knb = [SB(st, f"knb{i}", [128, 512], BF16) for i in range(2)]
                rs2 = [SB(st, f"rs2{i}", [128, 512], F32) for i in range(2)]
                c1 = [SB(st, f"c1{i}", [128, 512], F32) for i in range(2)]
                c2 = [SB(st, f"c2{i}", [128, 512], F32) for i in range(2)]
                rl = [SB(st, f"rl{i}", [128, 512], F32) for i in range(3)]
                dt_ = [SB(st, f"dt{i}", [128, 512], F32) for i in range(2)]
                dbuf = SB(st, "dbuf", [128, 2, 512], F32)
                R_w1, R_kv, R_rs, R_dbuf = (Res() for _ in range(4))
                R_q6 = [Res(), Res()]
                R_yb = [Res(), Res()]
                R_w2 = R_w1
                R_wo = R_w1
                R_xb = [Res()]
                R_hb = [Res()]
                R_tabs = [Res()]
                R_PT = [Res() for _ in range(4)]
                R_sq = [Res(), Res()]
                R_tn = [Res(), Res()]
                R_t0 = [Res(), Res()]
                R_knb = [Res(), Res()]
                R_rs2 = [Res(), Res()]
                R_c1 = [Res(), Res()]
                R_c2 = [Res(), Res()]
                R_rl = [Res() for _ in range(3)]
                R_dt = [Res(), Res()]
                nbufs = (sqb, R_sq, rs, R_rs, tn, R_tn, 0)

                wv = w_in_d[l].rearrange("(k p) n -> p k n", p=128)

                def wload(dst, R, off, lo, hi):
                    P.add("pool", lambda e: e.dma_start(out=dst[:, :, off:off + (hi - lo)], in_=wv[:, :, lo:hi]),
                          writes=[R], dma=True)
                wload(w1, R_w1, 0, 512, 576)
                wload(w1, R_w1, 64, 512, 576)
                wload(w1, R_w1, 128, 576, 640)
                wload(w1, R_w1, 192, 576, 640)
                wload(w1, R_w1, 256, 1792, 2048)
                wload(w1, R_w1, 512, 768, 1024)
                wload(w1, R_w1, 768, 1280, 1536)
                wload(w1, R_w1, 1024, 640, 768)
                wload(w1, R_w1, 1152, 2048, 2304)

                for blk in (0, 2, 4, 6, 9):
                    P.add("pool", lambda e, blk=blk: e.memset(Vb[:, :, blk, :], 1.0), writes=[R_kv])
                for (a_, b_) in ((0, 1), (2049, 2051), (2307, 2308)):
                    P.add("pool", lambda e, a_=a_, b_=b_: e.memset(zb[:, :, a_:b_], 0.0), writes=[R_kv])

                def zcol(t):
                    return t + 1 if t < NLAT else t + 3

                bsel_of = lambda b: (s if b < 4 else 2)
                xcnt = [0]

                def load_x(b):
                    t_0, nb = BLOCKS[b]
                    a = 0
                    P.add("sp", lambda e: e.dma_start(out=xb[a][:, :, :nb], in_=xscr[s][:, :, t_0:t_0 + nb]),
                          reads=[R_xscr[s]], writes=[R_xb[a]], dma=True)
                    return a

                tcnt = [0]

                def load_tabs(b):
                    t_0, nb = BLOCKS[b]
                    a = 0
                    P.add("sp", lambda e: e.dma_start(out=tabs[a][:, :, :nb],
                                                      in_=rope_d[:, :, t_0:t_0 + nb].rearrange("f p t -> p f t")),
                          writes=[R_tabs[a]], dma=True)
                    return a

                pcnt = [0]

                def qk_post(bi, nb, kind, gain_ap, rope, ta, out_ap, R_out):
                    i = pcnt[0] % 2
                    pcnt[0] += 1
                    bk = banks[bi]
                    hd, bdm, rotm, tb = (64, bd64_b, rotA_b, 0) if kind == "A" else (32, bd32_b, rotD_b, 2)
                    P.add("act", lambda e: e.mul(out=t0[i][:, :nb], in_=bk[:, :nb], mul=gain_ap),
                          reads=[R_bank[bi], R_small], writes=[R_t0[i]])
                    yield
                    P.add("act", lambda e: e.activation(out=sqb[i][:, :nb], in_=bk[:, :nb], func=AF.Square),
                          reads=[R_bank[bi]], writes=[R_sq[i]])
                    for _ in range(GAP):
                        yield
                    P.add("pe", lambda e: e.matmul(bk[:, :nb], lhsT=bdm, rhs=sqb[i][:, :nb], start=True, stop=True),
                          reads=[R_sq[i], R_constb, R_t0[i]], writes=[R_bank[bi]])
                    yield
                    P.add("act", lambda e: e.activation(out=rs2[i][:, :nb], in_=bk[:, :nb], func=AF.Ln, scale=1.0 / hd, bias=EPS),
                          reads=[R_bank[bi]], writes=[R_rs2[i]])
                    yield
                    P.add("act", lambda e: e.activation(out=rs2[i][:, :nb], in_=rs2[i][:, :nb], func=AF.Exp, scale=-0.5),
                          reads=[R_rs2[i]], writes=[R_rs2[i]])
                    yield
                    if not rope:
                        P.add("dve", lambda e: e.tensor_tensor(out=out_ap, in0=t0[i][:, :nb], in1=rs2[i][:, :nb], op=ALU.mult),
                              reads=[R_t0[i], R_rs2[i]], writes=[R_out])
                        yield
                        return
                    P.add("dve", lambda e: e.tensor_tensor(out=knb[i][:, :nb], in0=t0[i][:, :nb], in1=rs2[i][:, :nb], op=ALU.mult),
                          reads=[R_t0[i], R_rs2[i]], writes=[R_knb[i]])
                    for _ in range(GAP):
                        yield
                    P.add("pe", lambda e: e.matmul(bk[:, :nb], lhsT=rotm, rhs=knb[i][:, :nb], start=True, stop=True),
                          reads=[R_knb[i], R_constb, R_rs2[i]], writes=[R_bank[bi]])
                    yield
                    P.add("dve", lambda e: e.tensor_tensor(out=c1[i][:, :nb], in0=knb[i][:, :nb], in1=tabs[ta][:, tb, :nb], op=ALU.mult),
                          reads=[R_knb[i], R_tabs[ta]], writes=[R_c1[i]])
                    yield
                    P.add("dve", lambda e: e.tensor_tensor(out=c2[i][:, :nb], in0=bk[:, :nb], in1=tabs[ta][:, tb + 1, :nb], op=ALU.mult),
                          reads=[R_bank[bi], R_tabs[ta]], writes=[R_c2[i]])
                    yield
                    P.add("dve", lambda e: e.tensor_tensor(out=out_ap, in0=c1[i][:, :nb], in1=c2[i][:, :nb], op=ALU.add),
                          reads=[R_c1[i], R_c2[i]], writes=[R_out])
                    yield

                def proj(wt, R_w, col, ha, nb, bi):
                    for k in range(8):
                        P.add("pe", lambda e, k=k: e.matmul(banks[bi][:, :nb], lhsT=wt[:, k, col:col + 128], rhs=hb[ha][:, k, :nb],
                                                            start=(k == 0), stop=(k == 7)),
                              reads=[R_w, R_hb[ha]], writes=[R_bank[bi]])
                        if k == 3:
                            yield
                    yield

                pj = [1, 2, 3, 4, 5, 6, 7]
                pjc = [0]

                def nextbank():
                    b_ = pj[pjc[0] % len(pj)]
                    pjc[0] += 1
                    return b_

                def pass1_block(b):
                    t_0, nb = BLOCKS[b]
                    xa = load_x(b)
                    rope = b < 4
                    ta = load_tabs(b) if rope else 0
                    ha = 0
                    bsel = bsel_of(b)
                    run(norm_block(nbufs, lambda k, xa=xa, nb=nb: xb[xa][:, k, :nb], R_xb[xa], nb, l, 0, bsel,
                                   lambda k, ha=ha, nb=nb: hb[ha][:, k, :nb], R_hb[ha]))
                    for c0 in (0, 2):
                        gens = []
                        for c in (c0, c0 + 1):
                            bi = nextbank()
                            run(proj(w1, R_w1, c * 128, ha, nb, bi))
                            if c < 2:
                                gens.append(qk_post(bi, nb, "A", sm("gkA", l, l + 1), rope, ta, kA[:, c, t_0:t_0 + nb], R_kv))
                            else:
                                gens.append(qk_post(bi, nb, "D", sm("gkD", l, l + 1), rope, ta, kD[:, c - 2, t_0:t_0 + nb], R_kv))
                        rr(gens)
                    for c in range(2):
                        bu_i = nextbank()
                        run(proj(w1, R_w1, 512 + c * 128, ha, nb, bu_i))
                        i = pcnt[0] % 2
                        pcnt[0] += 1
                        P.add("act", lambda e, i=i, bu_i=bu_i, nb=nb: e.activation(out=t0[i][:, :nb], in_=banks[bu_i][:, :nb], func=AF.Copy),
                              reads=[R_bank[bu_i]], writes=[R_t0[i]])
                        bc_i = nextbank()
                        run(proj(w1, R_w1, 768 + c * 128, ha, nb, bc_i))
                        zc = zcol(t_0)
                        P.add("dve", lambda e, i=i, bc_i=bc_i, nb=nb, c=c, zc=zc: e.tensor_tensor(
                            out=zb[:, c, zc:zc + nb], in0=banks[bc_i][:, :nb], in1=t0[i][:, :nb], op=ALU.mult),
                            reads=[R_bank[bc_i], R_t0[i]], writes=[R_kv])
                    for j in range(nb // 128):
                        ti = t_0 // 128 + j
                        bi = nextbank()
                        for k in range(8):
                            P.add("pe", lambda e, k=k, j=j, bi=bi, ha=ha: e.matmul(
                                banks[bi][:, 0:384], lhsT=hb[ha][:, k, j * 128:(j + 1) * 128], rhs=w1[:, k, 1024:1408],
                                start=(k == 0), stop=(k == 7)), reads=[R_w1, R_hb[ha]], writes=[R_bank[bi]])
                        P.add("act", lambda e, ti=ti, bi=bi: e.activation(
                            out=Vb[:, ti, 1:5:2, :], in_=banks[bi][:, 0:128].rearrange("p (h d) -> p h d", d=64), func=AF.Copy),
                            reads=[R_bank[bi]], writes=[R_kv])
                        P.add("act", lambda e, ti=ti, bi=bi: e.activation(
                            out=Vb[:, ti, 5:9:2, :], in_=banks[bi][:, 128:256].rearrange("p (h d) -> p h d", d=64), func=AF.Copy),
                            reads=[R_bank[bi]], writes=[R_kv])
                        P.add("act", lambda e, ti=ti, bi=bi: e.activation(
                            out=Vb[:, ti, 8:11:2, :], in_=banks[bi][:, 256:384].rearrange("p (h d) -> p h d", d=64), func=AF.Copy),
                            reads=[R_bank[bi]], writes=[R_kv])

                for b in range(5):
                    pass1_block(b)
                tap("kA", kA[:].rearrange("p c t -> p (c t)"), R_kv, BF16)
                tap("kD", kD[:].rearrange("p c t -> p (c t)"), R_kv, BF16)
                tap("zb", zb[:].rearrange("p c t -> p (c t)"), R_kv, BF16)
                tap("Vb", Vb[:].rearrange("p t b d -> p (t b d)"), R_kv, BF16)
                wload(wA, R_w1, 0, 0, 512)
                wload(wA, R_w1, 512, 1536, 1792)
                wload(wA, R_w1, 768, 1024, 1280)
                P.add("pool", lambda e: e.dma_start(out=wA[:, :, 1024:2048], in_=w_out_d[l].rearrange("(k p) n -> p k n", p=128)),
                      writes=[R_w1], dma=True)
                sbanks = [2, 3, 4]
                obanks = [5, 6, 7]
                pj2 = [0, 1]
                nblocks = 4 if last else 5
                def prologue(b):
                    t_0, nb = BLOCKS[b]
                    qa = b % 2
                    xa = load_x(b)
                    rope = b < 4
                    ta = load_tabs(b) if rope else 0
                    ha = 0
                    bsel = bsel_of(b)
                    yield
                    yield from norm_block(nbufs, lambda k: xb[xa][:, k, :nb], R_xb[xa], nb, l, 0, bsel,
                                          lambda k: hb[ha][:, k, :nb], R_hb[ha])
                    for _ in range(GAP):
                        yield
                    for c in range(6):
                        bi = pj2[c % 2]
                        yield from proj(w2, R_w2, c * 128, ha, nb, bi)
                        yield
                        if c < 4:
                            yield from qk_post(bi, nb, "A", sm("gqA", l, l + 1), rope, ta, q6[qa][:, c, :nb], R_q6[qa])
                        else:
                            yield from qk_post(bi, nb, "D", sm("gqD", l, l + 1), rope, ta, q6[qa][:, c, :nb], R_q6[qa])
                    tap("hb", hb[ha][:].rearrange("p k t -> p (k t)"), R_hb[ha], BF16)
                    tap("q6", q6[qa][:].rearrange("p k t -> p (k t)"), R_q6[qa], BF16)
                    co, _ = _off["conv"]
                    zc = zcol(t_0)
                    for c in range(2):
                        bi = pj2[c % 2]
                        yield from proj(w2, R_w2, 768 + c * 128, ha, nb, bi)
                        i = pcnt[0] % 2
                        pcnt[0] += 1
                        wcol = lambda j, c=c: small[:, co + l * 6 + j * 2 + c:co + l * 6 + j * 2 + c + 1]
                        P.add("dve", lambda e, i=i, c=c, wcol=wcol: e.tensor_scalar(
                            out=c1[i][:, :nb], in0=zb[:, c, zc:zc + nb], scalar1=wcol(1), scalar2=None, op0=ALU.mult),
                            reads=[R_kv, R_small], writes=[R_c1[i]])
                        yield
                        P.add("dve", lambda e, i=i, c=c, wcol=wcol: e.scalar_tensor_tensor(
                            out=c2[i][:, :nb], in0=zb[:, c, zc - 1:zc - 1 + nb], scalar=wcol(0), in1=c1[i][:, :nb],
                            op0=ALU.mult, op1=ALU.add), reads=[R_kv, R_small, R_c1[i]], writes=[R_c2[i]])
                        yield
                        P.add("dve", lambda e, i=i, c=c, wcol=wcol: e.scalar_tensor_tensor(
                            out=c1[i][:, :nb], in0=zb[:, c, zc + 1:zc + 1 + nb], scalar=wcol(2), in1=c2[i][:, :nb],
                            op0=ALU.mult, op1=ALU.add), reads=[R_kv, R_small, R_c2[i]], writes=[R_c1[i]])
                        yield
                        P.add("dve", lambda e, i=i, c=c, bi=bi: e.tensor_tensor(
                            out=yb[qa][:, 4 + c, :nb], in0=banks[bi][:, :nb], in1=c1[i][:, :nb], op=ALU.mult),
                            reads=[R_bank[bi], R_c1[i]], writes=[R_yb[qa]])
                        yield

                def epilogue(b):
                    t_0, nb = BLOCKS[b]
                    qa = b % 2
                    bsel = bsel_of(b)
                    xa = load_x(b)
                    yield
                    for oc in range(8):
                        bi = pj2[oc % 2]
                        for c in range(8):
                            P.add("pe", lambda e, c=c, oc=oc, bi=bi: e.matmul(banks[bi][:, :nb], lhsT=wo[:, c, oc * 128:(oc + 1) * 128],
                                                                              rhs=yb[qa][:, c, :nb], start=(c == 0), stop=(c == 7)),
                                  reads=[R_wo, R_yb[qa]], writes=[R_bank[bi]])
                            if c == 3:
                                yield
                        yield
                        yield
                        P.add("dve", lambda e, oc=oc, bi=bi: e.scalar_tensor_tensor(
                            out=xb[xa][:, oc, :nb], in0=banks[bi][:, :nb], scalar=modT[:, l, 16 + oc, bsel:bsel + 1],
                            in1=xb[xa][:, oc, :nb], op0=ALU.mult, op1=ALU.add),
                            reads=[R_bank[bi], R_xb[xa], R_modT], writes=[R_xb[xa]])
                        yield
                    P.add("sp", lambda e: e.dma_start(out=xscr[s][:, :, t_0:t_0 + nb], in_=xb[xa][:, :, :nb]),
                          reads=[R_xb[xa]], writes=[R_xscr[s]], dma=True)
                    tap("x1", xb[xa][:].rearrange("p k t -> p (k t)"), R_xb[xa])
                    yield

                def attention(b, pending):
                    t_0, nb = BLOCKS[b]
                    qa = b % 2
                    q6c, ybc, R_q6c, R_ybc = q6[qa], yb[qa], R_q6[qa], R_yb[qa]

                    def pump():
                        while pending:
                            try:
                                next(pending[0])
                                return
                            except StopIteration:
                                pending.pop(0)

                    ktiles = list(range(NTILE)) if b < 4 else [16, 17]
                    groups = []
                    for h in range(8):
                        groups.append(("A", h, 0))
                    for h in range(4):
                        groups.append(("D", h, 0))
                        groups.append(("D", h, 1))
                    steps = [(gi, kt) for gi in range(len(groups)) for kt in ktiles]
                    obank_of = {}
                    rl_cnt = [0]

                    def emit_S(idx):
                        gi, kt = steps[idx]
                        kind, h, cc = groups[gi]
                        sb_i = sbanks[idx % 3]
                        if kind == "A":
                            par = h % 2
                            p0, pn = par * 64, 64
                            kv = h // 4
                            lhs = kA[p0:p0 + pn, kv, kt * 128:(kt + 1) * 128]
                            rhs = q6c[p0:p0 + pn, h // 2, :nb]
                        else:
                            p0, pn = (h % 2) * 64 + cc * 32, 32
                            lhs = kD[p0:p0 + pn, h // 2, kt * 128:(kt + 1) * 128]
                            rhs = q6c[p0:p0 + pn, 4 + h // 2, :nb]
                        kw = {}
                        if p0 == 96:
                            kw["tile_position"] = (96, 0)
                        P.add("pe", lambda e: e.matmul(banks[sb_i][:, :nb], lhsT=lhs, rhs=rhs, start=True, stop=True, **kw),
                              reads=[R_kv, R_q6c], writes=[R_bank[sb_i]])
                        pt_i = idx % 4
                        if kind == "A":
                            sc, bias = 0.125, nbA[:, l:l + 1]
                        else:
                            sc, bias = 32.0 ** -0.5, nbD[:, l:l + 1]
                        P.add("act", lambda e: e.activation(out=PT[pt_i][:, :nb], in_=banks[sb_i][:, :nb], func=AF.Exp,
                                                            scale=sc, bias=bias),
                              reads=[R_bank[sb_i], R_par], writes=[R_PT[pt_i]])

                    def emit_PV(idx):
                        gi, kt = steps[idx]
                        kind, h, cc = groups[gi]
                        first = kt == ktiles[0]
                        lastk = kt == ktiles[-1]
                        if first:
                            obank_of[gi] = obanks[gi % 3]
                        ob = obank_of[gi]
                        if kind == "A":
                            kv = h // 4
                            blk = (2 * kv + 1) if h % 2 == 0 else (2 * kv)
                        else:
                            blk = (5, 6, 8, 9)[h]
                        lhs = Vb[:, kt, blk:blk + 2, :].rearrange("p b d -> p (b d)")
                        pt_i = idx % 4
                        P.add("pe", lambda e: e.matmul(banks[ob][:, :nb], lhsT=lhs, rhs=PT[pt_i][:, :nb], start=first, stop=lastk),
                              reads=[R_kv, R_PT[pt_i]], writes=[R_bank[ob]])
                        if lastk:
                            finalize(gi)

                    def finalize(gi):
                        kind, h, cc = groups[gi]
                        ob = obank_of[gi]
                        par = h % 2
                        orow, lrow = par * 64, (1 - par) * 64
                        ri = rl_cnt[0] % 3
                        rl_cnt[0] += 1
                        P.add("dve", lambda e: e.reciprocal(out=rl[ri][orow:orow + 64, :nb], in_=banks[ob][lrow:lrow + 64, :nb]),
                              reads=[R_bank[ob]], writes=[R_rl[ri]])
                        if kind == "A":
                            P.add("dve", lambda e: e.tensor_tensor(out=ybc[orow:orow + 64, h // 2, :nb], in0=banks[ob][orow:orow + 64, :nb],
                                                                   in1=rl[ri][orow:orow + 64, :nb], op=ALU.mult),
                                  reads=[R_bank[ob], R_rl[ri]], writes=[R_ybc])
                            return
                        P.add("dve", lambda e: e.tensor_tensor(out=dt_[cc][orow:orow + 64, :nb], in0=banks[ob][orow:orow + 64, :nb],
                                                               in1=rl[ri][orow:orow + 64, :nb], op=ALU.mult),
                              reads=[R_bank[ob], R_rl[ri]], writes=[R_dt[cc]])
                        if cc == 1:
                            P.add("dve", lambda e: e.scalar_tensor_tensor(
                                out=dbuf[orow:orow + 64, h // 2, :nb], in0=dt_[1][orow:orow + 64, :nb],
                                scalar=neglam[orow:orow + 64, l:l + 1], in1=dt_[0][orow:orow + 64, :nb],
                                op0=ALU.mult, op1=ALU.add), reads=[R_dt[0], R_dt[1], R_par], writes=[R_dbuf])
                            if h % 2 == 1:
                                subln(h // 2, ob)

                    def subln(c, bi):
                        P.add("act", lambda e: e.activation(out=dt_[1][:, :nb], in_=dbuf[:, c, :nb], func=AF.Square),
                              reads=[R_dbuf], writes=[R_dt[1]])
                        P.add("pe", lambda e: e.matmul(banks[bi][:, :nb], lhsT=constf[:, 128:256], rhs=dt_[1][:, :nb], start=True, stop=True),
                              reads=[R_dt[1], R_constf], writes=[R_bank[bi]])
                        P.add("act", lambda e: e.activation(out=dt_[0][:, :nb], in_=banks[bi][:, :nb], func=AF.Ln, scale=1.0 / 64, bias=EPS),
                              reads=[R_bank[bi]], writes=[R_dt[0]])
                        P.add("act", lambda e: e.activation(out=dt_[0][:, :nb], in_=dt_[0][:, :nb], func=AF.Exp, scale=-0.5),
                              reads=[R_dt[0]], writes=[R_dt[0]])
                        P.add("dve", lambda e: e.scalar_tensor_tensor(
                            out=ybc[:, 6 + c, :nb], in0=dbuf[:, c, :nb], scalar=subws[:, l:l + 1], in1=dt_[0][:, :nb],
                            op0=ALU.mult, op1=ALU.mult), reads=[R_dbuf, R_dt[0], R_par], writes=[R_ybc])

                    LOOK = 2
                    n = len(steps)
                    for idx in range(min(LOOK, n)):
                        emit_S(idx)
                    for idx in range(n):
                        if idx + LOOK < n:
                            emit_S(idx + LOOK)
                        emit_PV(idx)
                        pump()
                    while pending:
                        pump()
                    tap("yb", ybc[:].rearrange("p k t -> p (k t)"), R_ybc, BF16)

                run(prologue(0))
                for b in range(nblocks):
                    pend = []
                    if b >= 1:
                        pend.append(epilogue(b - 1))
                    if b + 1 < nblocks:
                        pend.append(prologue(b + 1))
                    attention(b, pend)
                run(epilogue(nblocks - 1))
                barrier()

        def ffn(s, l, last, final):
            moe = (l % 2 == 1)
            li = l // 2
            HS = 3
            slabs = [(0, 3), (3, 3), (6, 3), (9, 3), (12, 3), (15, 3), (18, 2), (20, 2)]
            nblocks = 4 if last else 5
            with ExitStack() as st:
                facc = SB(st, "facc", [128, 8, NT], F32)
                h2 = SB(st, "h2", [128, 8, NT], BF16)
                wgu = [SB(st, f"wgu{i}", [128, 8, 2, HS * 128], BF16) for i in range(2)]
                wd = [SB(st, f"wd{i}", [128, HS, D], BF16) for i in range(2)]
                act = [SB(st, f"act{i}", [128, HS, 512], BF16) for i in range(2)]
                sg = [SB(st, f"sg{i}", [128, 512], F32) for i in range(2)]
                tt = [SB(st, f"tt{i}", [128, 512], F32) for i in range(2)]
                sqb = [SB(st, f"fsqb{i}", [128, 512], BF16) for i in range(2)]
                rs = SB(st, "frs", [128, 512], F32)
                tn = [SB(st, f"ftn{i}", [128, 512], F32) for i in range(2)]
                R_facc = [Res() for _ in range(5)]
                R_h2 = [Res() for _ in range(5)]
                R_wgu = [Res(), Res()]
                R_wd = [Res(), Res()]
                R_act = [Res(), Res()]
                R_sg = [Res(), Res()]
                R_tt = [Res(), Res()]
                R_sq = [Res(), Res()]
                R_tn = [Res(), Res()]
                R_rs = Res()
                nbufs = (sqb, R_sq, rs, R_rs, tn, R_tn, 6)
                if moe:
                    comb = SB(st, "comb", [128, NTILE, 8], F32)
                    lg = SB(st, "lg", [128, 4, 8], F32)
                    mx = SB(st, "mx", [128, 8], F32)
                    gt = SB(st, "gt", [128, 8], F32)
                    cc1 = SB(st, "cc1", [128, 8], F32)
                    cc2 = SB(st, "cc2", [128, 8], F32)
                    cwb = [SB(st, f"cwb{i}", [128, NT], BF16) for i in range(2)]
                    R_comb, R_lg, R_rt = Res(), Res(), Res()
                    R_cwb = [Res(), Res()]

                if moe:
                    wlist = [(mwg_d[li, e], mwu_d[li, e], mwd_d[li, e], e, h0, hs) for e in range(NE) for (h0, hs) in slabs]
                else:
                    wlist = [(dwg_d[li], dwu_d[li], dwd_d[li], 0, h0, hs) for (h0, hs) in slabs]

                def load_slab(i):
                    g_d, u_d, d_d, e_, h0, hs = wlist[i]
                    a = i % 2
                    gv = g_d.rearrange("(k p) n -> p k n", p=128)
                    uv = u_d.rearrange("(k p) n -> p k n", p=128)
                    dv = d_d.rearrange("(k p) n -> p k n", p=128)
                    P.add("pool", lambda e: e.dma_start(out=wgu[a][:, :, 0, :hs * 128], in_=gv[:, :, h0 * 128:(h0 + hs) * 128]),
                          writes=[R_wgu[a]], dma=True)
                    P.add("pool", lambda e: e.dma_start(out=wgu[a][:, :, 1, :hs * 128], in_=uv[:, :, h0 * 128:(h0 + hs) * 128]),
                          writes=[R_wgu[a]], dma=True)
                    P.add("pool", lambda e: e.dma_start(out=wd[a][:, :hs, :], in_=dv[:, h0:h0 + hs, :]),
                          writes=[R_wd[a]], dma=True)

                load_slab(0)
                if len(wlist) > 1:
                    load_slab(1)

                for b in range(5 if not last else 4):
                    t_0, nb = BLOCKS[b]
                    P.add("sp", lambda e, t_0=t_0, nb=nb: e.dma_start(out=facc[:, :, t_0:t_0 + nb], in_=xscr[s][:, :, t_0:t_0 + nb]),
                          reads=[R_xscr[s]], writes=[R_facc[b]], dma=True)
                for b in range(nblocks):
                    t_0, nb = BLOCKS[b]
                    bsel = s if b < 4 else 2
                    if not moe:
                        run(norm_block(nbufs, lambda k, t_0=t_0, nb=nb: facc[:, k, t_0:t_0 + nb], R_facc[b], nb, l, 1, bsel,
                                       lambda k, t_0=t_0, nb=nb: h2[:, k, t_0:t_0 + nb], R_h2[b]))
                        continue
                    ro, _ = _off["rw"]

                    def h32cb(k, ap32, R32, nb=nb, li=li):
                        for j in range(nb // 128):
                            P.add("pe", lambda e, j=j: e.matmul(
                                banks[7][:, j * 8:(j + 1) * 8], lhsT=ap32[:, j * 128:(j + 1) * 128],
                                rhs=small[:, ro + li * 64 + k * 8:ro + li * 64 + k * 8 + 8], start=(k == 0 and j == 0), stop=(k == 7),
                                skip_group_check=True),
                                reads=[R32, R_small], writes=[R_bank[7]])
                    run(norm_block(nbufs, lambda k, t_0=t_0, nb=nb: facc[:, k, t_0:t_0 + nb], R_facc[b], nb, l, 1, bsel,
                                   lambda k, t_0=t_0, nb=nb: h2[:, k, t_0:t_0 + nb], R_h2[b], h32=h32cb))
                    nt_ = nb // 128
                    rbo, _ = _off["rb"]
                    P.add("dve", lambda e, nt_=nt_, li=li: e.tensor_tensor(
                        out=lg[:, :nt_, :], in0=banks[7][:, 0:nt_ * 8].rearrange("p (j e) -> p j e", e=8),
                        in1=small[:, rbo + li * 8:rbo + li * 8 + 8].unsqueeze(1).to_broadcast([128, nt_, 8]), op=ALU.add),
                        reads=[R_bank[7], R_small], writes=[R_lg])
                    for j in range(nt_):
                        ti = t_0 // 128 + j
                        P.add("dve", lambda e, j=j: e.max(out=mx[:], in_=lg[:, j, :]), reads=[R_lg], writes=[R_rt])
                        P.add("dve", lambda e: e.tensor_tensor(out=gt[:, 0:1], in0=mx[:, 1:2], in1=mx[:, 0:1], op=ALU.subtract),
                              reads=[R_rt], writes=[R_rt])
                        P.add("act", lambda e: e.activation(out=gt[:, 1:2], in_=gt[:, 0:1], func=AF.Exp), reads=[R_rt], writes=[R_rt])
                        P.add("dve", lambda e: e.tensor_scalar_add(out=gt[:, 2:3], in0=gt[:, 1:2], scalar1=1.0), reads=[R_rt], writes=[R_rt])
                        P.add("dve", lambda e: e.reciprocal(out=gt[:, 3:4], in_=gt[:, 2:3]), reads=[R_rt], writes=[R_rt])
                        P.add("dve", lambda e: e.tensor_tensor(out=gt[:, 4:5], in0=gt[:, 1:2], in1=gt[:, 3:4], op=ALU.mult),
                              reads=[R_rt], writes=[R_rt])
                        P.add("dve", lambda e, j=j: e.tensor_scalar(out=cc1[:], in0=lg[:, j, :], scalar1=mx[:, 0:1], scalar2=gt[:, 3:4],
                                                                    op0=ALU.is_equal, op1=ALU.mult), reads=[R_lg, R_rt], writes=[R_rt])
                        P.add("dve", lambda e, j=j: e.tensor_scalar(out=cc2[:], in0=lg[:, j, :], scalar1=mx[:, 1:2], scalar2=gt[:, 4:5],
                                                                    op0=ALU.is_equal, op1=ALU.mult), reads=[R_lg, R_rt], writes=[R_rt])
                        P.add("dve", lambda e, ti=ti: e.tensor_tensor(out=comb[:, ti, :], in0=cc1[:], in1=cc2[:], op=ALU.add),
                              reads=[R_rt], writes=[R_comb])

                if moe:
                    tap("comb", comb[:].rearrange("p t e -> p (t e)"), R_comb)
                    tap("lg", lg[:].rearrange("p t e -> p (t e)"), R_lg)
                    tap("h2m", h2[:, :, 0:512], R_h2[0], BF16)
                tap("h2", h2[:, :, 0:512], R_h2[0], BF16)
                tap("facc0", facc[:, :, 0:512], R_facc[0])
                units = [(i, b) for i in range(len(wlist)) for b in range(nblocks)]
                gcnt = [0]
                ocnt = [0]
                cur_e = [-1]

                def make_cwb(e_):
                    a = e_ % 2
                    for b in range(nblocks):
                        t_0, nb = BLOCKS[b]
                        for j in range(nb // 128):
                            ti = t_0 // 128 + j
                            P.add("pe", lambda e, ti=ti, j=j: e.matmul(
                                banks[7][:, j * 128:(j + 1) * 128], lhsT=comb[:, ti, e_:e_ + 1].to_broadcast([128, 128]),
                                rhs=identf, start=True, stop=True), reads=[R_comb, R_constf], writes=[R_bank[7]])
                        P.add("act", lambda e, t_0=t_0, nb=nb: e.activation(out=cwb[a][:, t_0:t_0 + nb], in_=banks[7][:, :nb], func=AF.Copy),
                              reads=[R_bank[7]], writes=[R_cwb[a]])

                def emit_GU(u):
                    i, b = units[u]
                    _, _, _, e_, h0, hs = wlist[i]
                    if moe and e_ != cur_e[0]:
                        cur_e[0] = e_
                        make_cwb(e_)
                    a = i % 2
                    t_0, nb = BLOCKS[b]
                    aa = u % 2
                    for hc in range(hs):
                        gi = gcnt[0] % 2
                        gcnt[0] += 1
                        gb, ub = gi, 2 + gi
                        for k in range(8):
                            P.add("pe", lambda e, k=k, hc=hc, gb=gb: e.matmul(
                                banks[gb][:, :nb], lhsT=wgu[a][:, k, 0, hc * 128:(hc + 1) * 128], rhs=h2[:, k, t_0:t_0 + nb],
                                start=(k == 0), stop=(k == 7)), reads=[R_wgu[a], R_h2[b]], writes=[R_bank[gb]])
                        for k in range(8):
                            P.add("pe", lambda e, k=k, hc=hc, ub=ub: e.matmul(
                                banks[ub][:, :nb], lhsT=wgu[a][:, k, 1, hc * 128:(hc + 1) * 128], rhs=h2[:, k, t_0:t_0 + nb],
                                start=(k == 0), stop=(k == 7)), reads=[R_wgu[a], R_h2[b]], writes=[R_bank[ub]])
                        P.add("act", lambda e, gi=gi, gb=gb: e.activation(out=sg[gi][:, :nb], in_=banks[gb][:, :nb], func=AF.Silu),
                              reads=[R_bank[gb]], writes=[R_sg[gi]])
                        if moe:
                            P.add("dve", lambda e, gi=gi, ub=ub: e.tensor_tensor(out=tt[gi][:, :nb], in0=banks[ub][:, :nb], in1=sg[gi][:, :nb],
                                                                                 op=ALU.mult), reads=[R_bank[ub], R_sg[gi]], writes=[R_tt[gi]])
                            P.add("dve", lambda e, gi=gi, hc=hc: e.tensor_tensor(
                                out=act[aa][:, hc, :nb], in0=tt[gi][:, :nb], in1=cwb[e_ % 2][:, t_0:t_0 + nb], op=ALU.mult),
                                reads=[R_tt[gi], R_cwb[e_ % 2]], writes=[R_act[aa]])
                        else:
                            P.add("dve", lambda e, gi=gi, ub=ub, hc=hc: e.tensor_tensor(
                                out=act[aa][:, hc, :nb], in0=banks[ub][:, :nb], in1=sg[gi][:, :nb], op=ALU.mult),
                                reads=[R_bank[ub], R_sg[gi]], writes=[R_act[aa]])

                def emit_D(u):
                    i, b = units[u]
                    _, _, _, e_, h0, hs = wlist[i]
                    a = i % 2
                    t_0, nb = BLOCKS[b]
                    aa = u % 2
                    bsel = s if b < 4 else 2
                    for oc in range(8):
                        ob = 4 + ocnt[0] % 2
                        ocnt[0] += 1
                        for hc in range(hs):
                            P.add("pe", lambda e, hc=hc, oc=oc, ob=ob: e.matmul(
                                banks[ob][:, :nb], lhsT=wd[a][:, hc, oc * 128:(oc + 1) * 128], rhs=act[aa][:, hc, :nb],
                                start=(hc == 0), stop=(hc == hs - 1)), reads=[R_wd[a], R_act[aa]], writes=[R_bank[ob]])
                        P.add("dve", lambda e, oc=oc, ob=ob: e.scalar_tensor_tensor(
                            out=facc[:, oc, t_0:t_0 + nb], in0=banks[ob][:, :nb], scalar=modT[:, l, 40 + oc, bsel:bsel + 1],
                            in1=facc[:, oc, t_0:t_0 + nb], op0=ALU.mult, op1=ALU.add),
                            reads=[R_bank[ob], R_facc[b], R_modT], writes=[R_facc[b]])
                    if b == nblocks - 1 and i + 2 < len(wlist):
                        load_slab(i + 2)

                nu = len(units)
                emit_GU(0)
                for u in range(nu):
                    if u + 1 < nu:
                        emit_GU(u + 1)
                    emit_D(u)

                if final:
                    yt = [SB(st, f"yt{i}", [128, D], F32) for i in range(1)]
                    R_yt = [Res()]
                    for i in range(16):
                        a = 0
                        b = i // 4
                        for k in range(8):
                            bi = k // 4
                            P.add("pe", lambda e, i=i, k=k, bi=bi: e.transpose(
                                banks[bi][:, (k % 4) * 128:(k % 4 + 1) * 128], facc[:, k, i * 128:(i + 1) * 128], identf),
                                reads=[R_facc[b], R_constf], writes=[R_bank[bi]])
                        P.add("act", lambda e, a=a: e.activation(out=yt[a][:, 0:512], in_=banks[0][:], func=AF.Copy),
                              reads=[R_bank[0]], writes=[R_yt[a]])
                        P.add("dve", lambda e, a=a: e.tensor_copy(out=yt[a][:, 512:1024], in_=banks[1][:]),
                              reads=[R_bank[1]], writes=[R_yt[a]])
                        od = P.add("sp", lambda e, a=a, i=i: e.dma_start(out=y_d[s, i * 128:(i + 1) * 128, :], in_=yt[a][:]),
                                   reads=[R_yt[a]], dma=True)
                        out_dmas.append(od)
                else:
                    for b in range(nblocks):
                        t_0, nb = BLOCKS[b]
                        P.add("sp", lambda e, t_0=t_0, nb=nb: e.dma_start(out=xscr[s][:, :, t_0:t_0 + nb], in_=facc[:, :, t_0:t_0 + nb]),
                              reads=[R_facc[b]], writes=[R_xscr[s]], dma=True)
                barrier()

        for s in seqs:
            ingest(s)
            for l in range(n_layers):
                last = (l == n_layers - 1)
                mixer(s, l, last)
                ffn(s, l, last, final=last)

        P.emit(nc, final_waits=out_dmas)
    return nc


def pack_small(core, inp):
    f = lambda a: np.asarray(a, dtype=np.float32)
    sm = np.zeros((128, NS), np.float32)

    def put(name, arr):
        o, w = _off[name]
        arr = np.ascontiguousarray(arr, dtype=np.float32).reshape(128, w)
        sm[:, o:o + w] = arr

    c = f(inp["c"])
    cv = np.stack([c[2 * core], c[2 * core + 1], f(inp["c_ctx"])], axis=0)
    put("cT", cv.reshape(3, 8, 128).transpose(2, 1, 0))
    put("b_ada", f(inp["b_ada"]).reshape(4, 48, 128).transpose(2, 0, 1))
    put("nmix", f(inp["norm_mix"]).reshape(4, 8, 128).transpose(2, 0, 1))
    put("nffn", f(inp["norm_ffn"]).reshape(4, 8, 128).transpose(2, 0, 1))
    put("gqA", np.tile(f(inp["a_q_gain"]), (1, 2)).T)
    put("gkA", np.tile(f(inp["a_k_gain"]), (1, 2)).T)
    put("gqD", np.tile(f(inp["d_q_gain"]), (1, 4)).T)
    put("gkD", np.tile(f(inp["d_k_gain"]), (1, 4)).T)
    put("subw", np.tile(f(inp["diff_subln"]), (1, 2)).T)
    put("conv", f(inp["conv_w"]).reshape(4, 3, 2, 128).transpose(3, 0, 1, 2))
    put("lam", np.broadcast_to(f(inp["diff_lambda"]).reshape(1, 512), (128, 512)))
    put("rb", np.broadcast_to(f(inp["router_b"]).reshape(1, 16), (128, 16)))
    put("rw", f(inp["router_w"]).reshape(2, 8, 128, 8).transpose(2, 0, 1, 3))
    put("aq_rep", np.broadcast_to(f(inp["a_q_gain"]).reshape(1, 256), (128, 256)))
    put("ak_rep", np.broadcast_to(f(inp["a_k_gain"]).reshape(1, 256), (128, 256)))
    put("dq_rep", np.broadcast_to(f(inp["d_q_gain"]).reshape(1, 128), (128, 128)))
    put("dk_rep", np.broadcast_to(f(inp["d_k_gain"]).reshape(1, 128), (128, 128)))
    return sm


_SHARED = ("w_ada", "w_in", "w_out", "dense_w_gate", "dense_w_up", "dense_w_down",
           "moe_w_gate", "moe_w_up", "moe_w_down")


def make_in_maps(inp, cores):
    consts = make_consts()
    rope = make_rope()
    shared = {k: np.ascontiguousarray(np.asarray(inp[k], dtype=np.float32)) for k in _SHARED}
    x = np.asarray(inp["x"], dtype=np.float32)
    ctx = np.asarray(inp["ctx"], dtype=np.float32)
    maps = []
    for core in cores:
        m = dict(shared)
        m["x"] = np.ascontiguousarray(x[2 * core:2 * core + 2])
        m["ctx"] = np.ascontiguousarray(ctx[2 * core:2 * core + 2])
        m["small"] = pack_small(core, inp)
        m["consts"] = consts
        m["rope"] = rope
        maps.append(m)
    return maps


def kernel(**inputs):
    nc = build_program()
    in_maps = make_in_maps(inputs, list(range(8)))
    res = run_bass_kernel_spmd(nc, in_maps, core_ids=list(range(8)))
    out = np.concatenate([np.asarray(r["y"], dtype=np.float32) for r in res.results], axis=0)
    return out
```
